# Optimizing a Trainium2 kernel written in Bass

```python
import jax, jax.numpy as jnp
from jax import lax
import numpy as np

D_MODEL = 1024
BATCH = 8
SEQ = 2048
DEPTH = 4

N_EVEN = (DEPTH + 1) // 2
N_ODD = DEPTH // 2

N_Q_HEADS = 8
N_KV_HEADS = 2
HEAD_DIM = 64
Q_GROUP = N_Q_HEADS // N_KV_HEADS
WINDOW = 128
ATTN_BLOCK = 128
ALIBI_MAX = 8.0
CONV_WIDTH_B = D_MODEL // 2
CONV_GROUPS_B = 8
CONV_K_B = 3
Q_DIM = N_Q_HEADS * HEAD_DIM
KV_DIM = N_KV_HEADS * HEAD_DIM
EVEN_IN = Q_DIM + 2 * KV_DIM + 3 * CONV_WIDTH_B
EVEN_MIX = Q_DIM + CONV_WIDTH_B
EVEN_SPLITS = (Q_DIM, Q_DIM + KV_DIM, Q_DIM + 2 * KV_DIM, Q_DIM + 2 * KV_DIM + CONV_WIDTH_B, Q_DIM + 2 * KV_DIM + 2 * CONV_WIDTH_B)
LRU_WIDTH = D_MODEL
LRU_HEADS = 8
LRU_HEAD_DIM = LRU_WIDTH // LRU_HEADS
CONV_K_C = 4
LRU_C = 8.0
D_FF = (D_MODEL * 7) // 2
N_EXPERTS = 8
TOP_K = 2
MOE_BLOCK = 128
NORM_EPS = 1e-6
NEG_INF = -1e30

kernel_name = 'hybrid_swa_shortconv_rglru_moe'


def rms_norm(x, g):
    xf = x.astype(jnp.float32)
    y = xf * lax.rsqrt(jnp.mean(xf * xf, axis=-1, keepdims=True) + NORM_EPS)
    return (y * g.astype(jnp.float32)).astype(x.dtype)


def causal_depthwise_conv(x, w):
    K = w.shape[0]
    S = x.shape[1]
    xp = jnp.pad(x, ((0, 0), (K - 1, 0), (0, 0)))
    return sum(xp[:, k:k + S] * w[k] for k in range(K))


def alibi_slopes():
    h = jnp.arange(1, N_Q_HEADS + 1, dtype=jnp.float32)
    return jnp.exp2(-ALIBI_MAX * h / N_Q_HEADS)


def sliding_window_attention(q, k, v, sinks):
    B, S = q.shape[0], q.shape[1]
    L = ATTN_BLOCK
    nb = S // L
    qb = q.reshape(B, nb, L, N_KV_HEADS, Q_GROUP, HEAD_DIM)

    def band(t):
        cur = t.reshape(B, nb, L, N_KV_HEADS, HEAD_DIM)
        prev = jnp.pad(cur, ((0, 0), (1, 0), (0, 0), (0, 0), (0, 0)))[:, :-1]
        return jnp.concatenate([prev, cur], axis=2)

    kb, vb = band(k), band(v)
    scores = jnp.einsum('bnqkgd,bnskd->bnkgqs', qb, kb, preferred_element_type=jnp.float32) * (HEAD_DIM ** -0.5)
    qi = jnp.arange(L)[:, None]
    sj = jnp.arange(2 * L)[None, :]
    diff = L + qi - sj
    s_abs = (jnp.arange(nb)[:, None] - 1) * L + jnp.arange(2 * L)[None, :]
    valid = (diff >= 0)[None] & (diff < WINDOW)[None] & (s_abs >= 0)[:, None, :]
    slopes = alibi_slopes().reshape(N_KV_HEADS, Q_GROUP)
    bias = -slopes[:, :, None, None] * diff.astype(jnp.float32)
    scores = jnp.where(valid[None, :, None, None], scores + bias, NEG_INF)
    sink = jnp.broadcast_to(sinks.astype(jnp.float32).reshape(N_KV_HEADS, Q_GROUP)[None, None, :, :, None, None], scores.shape[:-1] + (1,))
    probs = jax.nn.softmax(jnp.concatenate([scores, sink], axis=-1), axis=-1)[..., :-1]
    out = jnp.einsum('bnkgqs,bnskd->bnqkgd', probs.astype(v.dtype), vb)
    return out.reshape(B, S, Q_DIM)


def attn_conv_mixer(h, w_in, q_gain, k_gain, sinks, conv_w, w_out):
    B, S, _ = h.shape
    q, k, v, gate_b, gate_c, u = jnp.split(h @ w_in, EVEN_SPLITS, axis=-1)
    q = rms_norm(q.reshape(B, S, N_Q_HEADS, HEAD_DIM), q_gain)
    k = rms_norm(k.reshape(B, S, N_KV_HEADS, HEAD_DIM), k_gain)
    v = v.reshape(B, S, N_KV_HEADS, HEAD_DIM)
    attn = sliding_window_attention(q, k, v, sinks)
    conv = gate_b * causal_depthwise_conv(gate_c * u, conv_w)
    return jnp.concatenate([attn, conv], axis=-1) @ w_out


def recurrent_mixer(h, w_in, conv_w, conv_b, ga_w, ga_b, gx_w, gx_b, lam, w_out):
    B, S, _ = h.shape
    y_branch, x_branch = jnp.split(h @ w_in, 2, axis=-1)
    y_branch = jax.nn.gelu(y_branch)
    xc = causal_depthwise_conv(x_branch, conv_w) + conv_b
    xh = xc.reshape(B, S, LRU_HEADS, LRU_HEAD_DIM)
    r = jax.nn.sigmoid(jnp.einsum('bshi,hij->bshj', xh, ga_w).reshape(B, S, LRU_WIDTH) + ga_b)
    i = jax.nn.sigmoid(jnp.einsum('bshi,hij->bshj', xh, gx_w).reshape(B, S, LRU_WIDTH) + gx_b)
    log_a = -LRU_C * r.astype(jnp.float32) * jax.nn.softplus(-lam.astype(jnp.float32))
    a = jnp.exp(log_a)
    mult = jnp.sqrt(jnp.maximum(-jnp.expm1(2.0 * log_a), 0.0))
    mult = jnp.where(jnp.arange(S)[None, :, None] == 0, 1.0, mult)
    b = mult * (i * xc).astype(jnp.float32)

    def combine(c1, c2):
        a1, b1 = c1
        a2, b2 = c2
        return a1 * a2, a2 * b1 + b2

    _, hs = lax.associative_scan(combine, (a, b), axis=1)
    return (y_branch * hs.astype(h.dtype)) @ w_out


def swiglu(h, w_gate, w_up, w_down):
    return (jax.nn.silu(h @ w_gate) * (h @ w_up)) @ w_down


def moe_swiglu(h, w_router, w_gate, w_up, w_down):
    B, S, D = h.shape
    T = B * S
    xf = h.reshape(T, D)
    logits = (xf @ w_router).astype(jnp.float32)
    top_logits, top_idx = lax.top_k(logits, TOP_K)
    gates = jax.nn.softmax(top_logits, axis=-1)
    n_assign = T * TOP_K
    e_flat = top_idx.reshape(-1).astype(jnp.int32)
    tok_flat = jnp.repeat(jnp.arange(T, dtype=jnp.int32), TOP_K)
    g_flat = gates.reshape(-1)
    order = jnp.argsort(e_flat)
    e_sorted = e_flat[order]
    counts = jnp.zeros((N_EXPERTS,), jnp.int32).at[e_flat].add(1)
    padded = (counts + MOE_BLOCK - 1) // MOE_BLOCK * MOE_BLOCK
    pad_end = jnp.cumsum(padded)
    pad_start = pad_end - padded
    start = jnp.cumsum(counts) - counts
    dest = pad_start[e_sorted] + jnp.arange(n_assign, dtype=jnp.int32) - start[e_sorted]
    n_rows = n_assign + N_EXPERTS * MOE_BLOCK
    n_blocks = n_rows // MOE_BLOCK
    row_tok = jnp.zeros((n_rows,), jnp.int32).at[dest].set(tok_flat[order])
    row_gate = jnp.zeros((n_rows,), jnp.float32).at[dest].set(g_flat[order])
    blk_start = jnp.arange(n_blocks, dtype=jnp.int32) * MOE_BLOCK
    blk_expert = jnp.minimum(jnp.searchsorted(pad_end, blk_start, side='right'), N_EXPERTS - 1)

    def expert_block(args):
        tok, e = args
        xb = xf[tok]
        hb = jax.nn.silu(xb @ w_gate[e]) * (xb @ w_up[e])
        return hb @ w_down[e]

    yb = lax.map(expert_block, (row_tok.reshape(n_blocks, MOE_BLOCK), blk_expert))
    y = yb.reshape(n_rows, D).astype(jnp.float32) * row_gate[:, None]
    out = jnp.zeros((T, D), jnp.float32).at[row_tok].add(y)
    return out.astype(h.dtype).reshape(B, S, D)


def setup_inputs(seed: int = 0) -> dict:
    key = jax.random.key(seed)
    ks = iter(jax.random.split(key, 32))
    f32 = jnp.float32
    res_scale = (2 * DEPTH) ** -0.5

    def nrm(shape, scale):
        return jax.random.normal(next(ks), shape, f32) * scale

    x = nrm((BATCH, SEQ, D_MODEL), 1.0)
    norm_mix = 1.0 + nrm((DEPTH, D_MODEL), 0.02)
    norm_ffn = 1.0 + nrm((DEPTH, D_MODEL), 0.02)
    hy_w_in = nrm((N_EVEN, D_MODEL, EVEN_IN), D_MODEL ** -0.5)
    hy_q_gain = 1.0 + nrm((N_EVEN, HEAD_DIM), 0.02)
    hy_k_gain = 1.0 + nrm((N_EVEN, HEAD_DIM), 0.02)
    hy_sinks = nrm((N_EVEN, N_Q_HEADS), 0.5)
    hy_conv_w = nrm((N_EVEN, CONV_K_B, CONV_WIDTH_B), CONV_K_B ** -0.5)
    hy_w_out = nrm((N_EVEN, EVEN_MIX, D_MODEL), EVEN_MIX ** -0.5 * res_scale)
    rg_w_in = nrm((N_ODD, D_MODEL, 2 * LRU_WIDTH), D_MODEL ** -0.5)
    rg_conv_w = nrm((N_ODD, CONV_K_C, LRU_WIDTH), CONV_K_C ** -0.5)
    rg_conv_b = nrm((N_ODD, LRU_WIDTH), 0.01)
    rg_gate_a_w = nrm((N_ODD, LRU_HEADS, LRU_HEAD_DIM, LRU_HEAD_DIM), LRU_HEAD_DIM ** -0.5)
    rg_gate_a_b = nrm((N_ODD, LRU_WIDTH), 0.01)
    rg_gate_x_w = nrm((N_ODD, LRU_HEADS, LRU_HEAD_DIM, LRU_HEAD_DIM), LRU_HEAD_DIM ** -0.5)
    rg_gate_x_b = nrm((N_ODD, LRU_WIDTH), 0.01)
    a_pow_c = jax.random.uniform(next(ks), (N_ODD, LRU_WIDTH), f32, minval=0.9, maxval=0.999)
    a0 = a_pow_c ** (1.0 / LRU_C)
    rg_lambda = jnp.log(a0) - jnp.log1p(-a0)
    rg_w_out = nrm((N_ODD, LRU_WIDTH, D_MODEL), LRU_WIDTH ** -0.5 * res_scale)
    ffn_w_gate = nrm((N_EVEN, D_MODEL, D_FF), D_MODEL ** -0.5)
    ffn_w_up = nrm((N_EVEN, D_MODEL, D_FF), D_MODEL ** -0.5)
    ffn_w_down = nrm((N_EVEN, D_FF, D_MODEL), D_FF ** -0.5 * res_scale)
    moe_router = nrm((N_ODD, D_MODEL, N_EXPERTS), D_MODEL ** -0.5)
    moe_w_gate = nrm((N_ODD, N_EXPERTS, D_MODEL, D_FF), D_MODEL ** -0.5)
    moe_w_up = nrm((N_ODD, N_EXPERTS, D_MODEL, D_FF), D_MODEL ** -0.5)
    moe_w_down = nrm((N_ODD, N_EXPERTS, D_FF, D_MODEL), D_FF ** -0.5 * res_scale)
    return {'x': x, 'norm_mix': norm_mix, 'norm_ffn': norm_ffn,
            'hy_w_in': hy_w_in, 'hy_q_gain': hy_q_gain, 'hy_k_gain': hy_k_gain, 'hy_sinks': hy_sinks,
            'hy_conv_w': hy_conv_w, 'hy_w_out': hy_w_out,
            'rg_w_in': rg_w_in, 'rg_conv_w': rg_conv_w, 'rg_conv_b': rg_conv_b,
            'rg_gate_a_w': rg_gate_a_w, 'rg_gate_a_b': rg_gate_a_b, 'rg_gate_x_w': rg_gate_x_w,
            'rg_gate_x_b': rg_gate_x_b, 'rg_lambda': rg_lambda, 'rg_w_out': rg_w_out,
            'ffn_w_gate': ffn_w_gate, 'ffn_w_up': ffn_w_up, 'ffn_w_down': ffn_w_down,
            'moe_router': moe_router, 'moe_w_gate': moe_w_gate, 'moe_w_up': moe_w_up, 'moe_w_down': moe_w_down}


def reference(x, norm_mix, norm_ffn, hy_w_in, hy_q_gain, hy_k_gain, hy_sinks, hy_conv_w, hy_w_out,
              rg_w_in, rg_conv_w, rg_conv_b, rg_gate_a_w, rg_gate_a_b, rg_gate_x_w, rg_gate_x_b, rg_lambda, rg_w_out,
              ffn_w_gate, ffn_w_up, ffn_w_down, moe_router, moe_w_gate, moe_w_up, moe_w_down):
    for layer in range(DEPTH):
        j = layer // 2
        h = rms_norm(x, norm_mix[layer])
        if layer % 2 == 0:
            x = x + attn_conv_mixer(h, hy_w_in[j], hy_q_gain[j], hy_k_gain[j], hy_sinks[j], hy_conv_w[j], hy_w_out[j])
            h = rms_norm(x, norm_ffn[layer])
            x = x + swiglu(h, ffn_w_gate[j], ffn_w_up[j], ffn_w_down[j])
        else:
            x = x + recurrent_mixer(h, rg_w_in[j], rg_conv_w[j], rg_conv_b[j], rg_gate_a_w[j], rg_gate_a_b[j],
                                    rg_gate_x_w[j], rg_gate_x_b[j], rg_lambda[j], rg_w_out[j])
            h = rms_norm(x, norm_ffn[layer])
            x = x + moe_swiglu(h, moe_router[j], moe_w_gate[j], moe_w_up[j], moe_w_down[j])
    return x
```

```python
import numpy as np
from contextlib import ExitStack
import concourse.bass as bass
import concourse.mybir as mybir
from concourse.bass_utils import run_bass_kernel_spmd

F32 = mybir.dt.float32
BF16 = mybir.dt.bfloat16
AF = mybir.ActivationFunctionType
ALU = mybir.AluOpType
AX = mybir.AxisListType
P = 128
D = 1024
KC = 8
EPS = 1e-6
SLOT = 2048


class Cfg:
    def __init__(self, S=2048, DFF=3584, NE=8, depth=4, FG=2, nslots=6):
        self.S, self.DFF, self.NE, self.depth, self.FG, self.nslots = S, DFF, NE, depth, FG, nslots
        self.n_even = (depth + 1) // 2
        self.n_odd = depth // 2
        self.sparse = True
        self.CAP = 384


def param_layout(cfg):
    cols = {}
    o = 0
    for name, w in (("nm", cfg.depth * 8), ("nf", cfg.depth * 8), ("qg", cfg.n_even), ("kg", cfg.n_even),
                    ("cw", cfg.n_even * 12), ("rcw", cfg.n_odd * 32), ("rcb", cfg.n_odd * 8),
                    ("gab", cfg.n_odd * 8), ("gxb", cfg.n_odd * 8), ("lam", cfg.n_odd * 8),
                    ("snk", cfg.n_even * 8)):
        cols[name] = o
        o += w
    return cols, o


def prep_shared(cfg, inp):
    f = np.float32
    cols, npar = param_layout(cfg)
    prm = np.zeros((P, npar), f)

    def fm(v):
        v = np.asarray(v, f)
        C = v.shape[-1] // P
        return np.moveaxis(v.reshape(v.shape[:-1] + (C, P)), -1, 0)

    prm[:, cols["nm"]:cols["nm"] + cfg.depth * 8] = fm(inp["norm_mix"]).reshape(P, -1)
    prm[:, cols["nf"]:cols["nf"] + cfg.depth * 8] = fm(inp["norm_ffn"]).reshape(P, -1)
    prm[:, cols["qg"]:cols["qg"] + cfg.n_even] = np.tile(np.asarray(inp["hy_q_gain"], f).T, (2, 1))
    prm[:, cols["kg"]:cols["kg"] + cfg.n_even] = np.tile(np.asarray(inp["hy_k_gain"], f).T, (2, 1))
    prm[:, cols["cw"]:cols["cw"] + cfg.n_even * 12] = fm(inp["hy_conv_w"]).reshape(P, -1)
    if cfg.n_odd:
        prm[:, cols["rcw"]:cols["rcw"] + cfg.n_odd * 32] = fm(inp["rg_conv_w"]).reshape(P, -1)
        for nm_, key in (("rcb", "rg_conv_b"), ("gab", "rg_gate_a_b"), ("gxb", "rg_gate_x_b"), ("lam", "rg_lambda")):
            prm[:, cols[nm_]:cols[nm_] + cfg.n_odd * 8] = fm(inp[key]).reshape(P, -1)
    prm[:, cols["snk"]:cols["snk"] + cfg.n_even * 8] = np.broadcast_to(
        np.asarray(inp["hy_sinks"], f).reshape(1, -1), (P, cfg.n_even * 8))

    cbf = np.zeros((P, 3, P), f)
    cbf[:, 0] = np.eye(P, dtype=f)
    cbf[:, 1] = 1.0 / 1024.0
    cbf[0:64, 2, 0:64] = 1.0 / 64.0
    cbf[64:128, 2, 64:128] = 1.0 / 64.0
    cf = np.zeros((P, 3, P), f)
    cf[:, 0] = np.eye(P, dtype=f)
    cf[:, 1] = 1.0
    cf[:, 2] = np.triu(np.ones((P, P), f), 1)
    io = np.zeros((P, cfg.CAP + 4), f)
    io[:, 0:cfg.CAP] = np.arange(cfg.CAP, dtype=f)[None, :]
    for b_ in range(4):
        io[:, cfg.CAP + b_] = np.arange(P, dtype=f) + 128.0 * b_
    sj = np.arange(P)[:, None].astype(np.float64)
    qi = np.arange(P)[None, :].astype(np.float64)
    eb = np.zeros((P, 8, 256), np.float64)
    for h in range(8):
        slope = 2.0 ** (-(h + 1))
        dprev = 128.0 + qi - sj
        eb[:, h, 0:128] = np.where(dprev < 128.0, np.exp(-slope * dprev), 0.0)
        dcur = qi - sj
        eb[:, h, 128:256] = np.where(dcur >= 0.0, np.exp(-slope * dcur), 0.0)
    ebd = np.zeros((P, 4, 512), np.float64)
    for h in range(8):
        g_, hh_ = h // 4, h % 4
        ebd[:, 2 * g_ + hh_ % 2, (hh_ // 2) * 256:(hh_ // 2 + 1) * 256] = eb[:, h, :]
    sh = {"prm": prm, "cbf": cbf.reshape(P, -1), "cf": cf.reshape(P, -1), "eb": ebd.astype(f).reshape(P, -1), "io": io}

    w_in = np.asarray(inp["hy_w_in"], f)
    q, k, v, rest = w_in[..., 0:512], w_in[..., 512:640], w_in[..., 640:768], w_in[..., 768:]
    k0, k1 = k[..., 0:64], k[..., 64:128]
    sh["hy_w_in"] = np.ascontiguousarray(np.concatenate([q, k0, k0, k1, k1, v, rest], axis=-1))
    sh["hy_w_out"] = np.ascontiguousarray(inp["hy_w_out"], f)
    sh["ffn_w_gate"] = np.ascontiguousarray(inp["ffn_w_gate"], f)
    sh["ffn_w_up"] = np.ascontiguousarray(inp["ffn_w_up"], f)
    sh["ffn_w_down"] = np.ascontiguousarray(inp["ffn_w_down"], f)
    if cfg.n_odd:
        rw = np.asarray(inp["rg_w_in"], f)
        y, xx = rw[..., 0:1024].reshape(-1, D, 8, 128), rw[..., 1024:].reshape(-1, D, 8, 128)
        sh["rg_w_in"] = np.ascontiguousarray(np.stack([y, xx], axis=3).reshape(-1, D, 2048))
        sh["rg_ga"] = np.ascontiguousarray(inp["rg_gate_a_w"], f)
        sh["rg_gx"] = np.ascontiguousarray(inp["rg_gate_x_w"], f)
        sh["rg_w_out"] = np.ascontiguousarray(inp["rg_w_out"], f)
        r = np.asarray(inp["moe_router"], f).reshape(cfg.n_odd, KC, P, cfg.NE)
        sh["rtr"] = np.ascontiguousarray(np.transpose(r, (2, 0, 1, 3)).reshape(P, -1))
        sh["moe_w_gate"] = np.ascontiguousarray(inp["moe_w_gate"], f)
        sh["moe_w_up"] = np.ascontiguousarray(inp["moe_w_up"], f)
        sh["moe_w_down"] = np.ascontiguousarray(inp["moe_w_down"], f)
    for key in ("hy_w_in", "hy_w_out", "ffn_w_gate", "ffn_w_up", "ffn_w_down", "rg_w_in", "rg_ga", "rg_gx", "rg_w_out",
                "moe_w_gate", "moe_w_up", "moe_w_down"):
        if key in sh:
            a = sh.pop(key)
            for jj in range(a.shape[0]):
                sh["%s_%d" % (key, jj)] = np.ascontiguousarray(a[jj])
    return sh


class StopEmit(Exception):
    pass


class Buf:
    __slots__ = ("w", "r")

    def __init__(self):
        self.w = None
        self.r = {}


class Ctx:
    def __init__(self, nc, dry):
        self.nc = nc
        self.dry = dry
        self.eng = {"pe": nc.tensor, "act": nc.scalar, "dve": nc.vector, "pool": nc.gpsimd, "sp": nc.sync}
        self.sem = {}
        self.cnt = {}
        self.known = {e: {} for e in self.eng}
        for e in self.eng:
            self.new_sem(e)
        self.pi = 0
        self.reserved = set()
        self.nops = 0
        self.skip = False

    def new_sem(self, key):
        self.sem[key] = None if self.dry else self.nc.alloc_semaphore(name="sem_%s" % (key if isinstance(key, str) else "_".join(map(str, key))))
        self.cnt[key] = 0
        return key

    def _wait(self, e, deps):
        kn = self.known[e]
        for key, val in deps.items():
            if val <= 0 or kn.get(key, 0) >= val:
                continue
            self.eng[e].wait_ge(self.sem[key], val)
            kn[key] = val

    def _deps(self, e, reads, writes):
        deps = {}
        for b in reads:
            if b.w is not None and deps.get(b.w[0], 0) < b.w[1]:
                deps[b.w[0]] = b.w[1]
        for b in writes:
            if b.w is not None and deps.get(b.w[0], 0) < b.w[1]:
                deps[b.w[0]] = b.w[1]
            for k, v in b.r.items():
                if k != e and deps.get(k, 0) < v:
                    deps[k] = v
        if e == "pe":
            deps.pop("pe", None)
        return deps

    def op(self, e, fn, reads=(), writes=(), inc=True):
        if self.dry or self.skip:
            return
        self.nops += 1
        self._wait(e, self._deps(e, reads, writes))
        ins = fn(self.eng[e])
        if inc:
            self.cnt[e] += 1
            ins.then_inc(self.sem[e], 1)
            val = self.cnt[e]
        else:
            val = self.cnt[e] + 1
        for b in writes:
            b.w = (e, val)
            b.r = {}
        for b in reads:
            if b.r.get(e, 0) < val:
                b.r[e] = val

    def dma(self, q, out, in_, sem_key, reads=(), writes=()):
        if self.dry or self.skip:
            return
        self.nops += 1
        self._wait(q, self._deps("dma", reads, writes))
        ins = self.eng[q].dma_start(out=out, in_=in_)
        self.cnt[sem_key] += 16
        ins.then_inc(self.sem[sem_key], 16)
        val = self.cnt[sem_key]
        for b in writes:
            b.w = (sem_key, val)
            b.r = {}
        for b in reads:
            if b.r.get(sem_key, 0) < val:
                b.r[sem_key] = val

    def barrier(self):
        if self.dry or self.skip:
            return
        for e in self.eng:
            self._wait(e, dict(self.cnt))

    def ps_next(self, n=1):
        while True:
            if n == 2 and self.pi % 2:
                self.pi += 1
            b = self.pi % 8
            if any(((b + i) % 8) in self.reserved for i in range(n)):
                self.pi += 1
                continue
            self.pi += n
            return b


class WStream:
    def __init__(self, ctx, dr, ring, nslots, specs):
        self.ctx, self.dr, self.ring, self.n = ctx, dr, ring, nslots
        self.specs = specs
        self.rec = []
        self.i = 0
        self.issued = 0
        self.bufs = [Buf() for _ in range(nslots)]
        self.keys = [ctx.new_sem(("w", s)) for s in range(nslots)]

    def _views(self, spec, s):
        kind, name, pre = spec[0], spec[1], spec[2]
        t = self.dr["%s_%d" % (name, pre[0])]
        a = t[pre[1:]] if len(pre) > 1 else t
        slot = self.ring[:, s, :]
        if kind == "cols":
            c0, ncol = spec[3], spec[4]
            return a[:, c0:c0 + ncol].rearrange("(k p) c -> p k c", p=P), slot[:, 0:KC * ncol].rearrange("p (k c) -> p k c", k=KC)
        r0, nf = spec[3], spec[4]
        return a[r0:r0 + nf * P, :].rearrange("(f p) c -> p f c", p=P), slot[:, 0:nf * D].rearrange("p (f c) -> p f c", f=nf)

    def get(self, spec):
        if self.specs is None:
            self.rec.append(spec)
            s = (len(self.rec) - 1) % self.n
            return self._views(spec, s)[1], self.bufs[s]
        i = self.i
        self.i += 1
        assert self.specs[i] == spec, (i, self.specs[i], spec)
        if self.ctx.skip:
            return self._views(spec, i % self.n)[1], self.bufs[i % self.n]
        while self.issued < min(len(self.specs), i + self.n - 3):
            jj = self.issued
            s = jj % self.n
            src, dst = self._views(self.specs[jj], s)
            self.ctx.dma("pool", out=dst, in_=src, sem_key=self.keys[s], writes=[self.bufs[s]])
            self.issued += 1
        s = i % self.n
        return self._views(spec, s)[1], self.bufs[s]


def emit(nc, ctx, cfg, layers, specs):
    S = cfg.S
    NT = S // 512
    NB = S // 128
    NF = cfg.DFF // 128
    FG = cfg.FG
    NE = cfg.NE
    cols, npar = param_layout(cfg)
    es = ExitStack()

    def din(name, shape):
        return nc.dram_tensor(name, list(shape), F32, kind="ExternalInput").ap()

    dr = {"x": din("x", [S, D]), "prm": din("prm", [P, npar]), "cbf": din("cbf", [P, 3 * P]),
          "cf": din("cf", [P, 3 * P]), "eb": din("eb", [P, 2048]), "io": din("io", [P, cfg.CAP + 4])}
    for jj in range(cfg.n_even):
        for nm_, shp in (("hy_w_in", [D, 2432]), ("hy_w_out", [D, D]), ("ffn_w_gate", [D, cfg.DFF]),
                         ("ffn_w_up", [D, cfg.DFF]), ("ffn_w_down", [cfg.DFF, D])):
            dr["%s_%d" % (nm_, jj)] = din("%s_%d" % (nm_, jj), shp)
    if cfg.n_odd:
        dr["rtr"] = din("rtr", [P, cfg.n_odd * KC * NE])
    for jj in range(cfg.n_odd):
        for nm_, shp in (("rg_w_in", [D, 2048]), ("rg_ga", [8, P, P]), ("rg_gx", [8, P, P]), ("rg_w_out", [D, D]),
                         ("moe_w_gate", [NE, D, cfg.DFF]), ("moe_w_up", [NE, D, cfg.DFF]), ("moe_w_down", [NE, cfg.DFF, D])):
            dr["%s_%d" % (nm_, jj)] = din("%s_%d" % (nm_, jj), shp)
    out_d = nc.dram_tensor("out", [S, D], F32, kind="ExternalOutput").ap()

    uid = [0]

    def sb(stack, name, shape, dt):
        uid[0] += 1
        return stack.enter_context(nc.sbuf_tensor("%s_%d" % (name, uid[0]), shape, dt))

    xT = sb(es, "xT", [P, KC, S], F32)
    hT = sb(es, "hT", [P, KC, S], BF16)
    xb = [Buf() for _ in range(NT)]
    hb = [Buf() for _ in range(NT)]
    ps = es.enter_context(nc.psum_tensor("ps", [P, 8, 512], F32))
    pb = [Buf() for _ in range(8)]
    ring = sb(es, "ring", [P, cfg.nslots, SLOT], BF16)
    ws = WStream(ctx, dr, ring, cfg.nslots, specs)
    sq = sb(es, "sq", [P, 2, 2, 512], BF16)
    sqb = [Buf(), Buf()]
    rstd = sb(es, "rstd", [P, 2, 512], F32)
    rsb = [Buf(), Buf()]
    cbf = sb(es, "cbf_s", [P, 3, P], BF16)
    cf = sb(es, "cf_s", [P, 3, P], F32)
    io = sb(es, "io_s", [P, cfg.CAP + 4], F32)
    eb = sb(es, "eb_s", [P, 4, 512], F32)
    prm = sb(es, "prm_s", [P, npar], F32)
    rtr = sb(es, "rtr_s", [P, max(1, cfg.n_odd) * KC * NE], F32)
    der = sb(es, "der_s", [P, 96], F32)
    cB = Buf()
    dB = Buf()
    par = [0]

    def flip():
        par[0] ^= 1
        return par[0]

    def pc(name, i):
        return prm[:, cols[name] + i:cols[name] + i + 1]

    csem = ctx.new_sem("cst")
    ctx.dma("sp", out=cf[:, :, :], in_=dr["cf"].rearrange("p (a b) -> p a b", a=3), sem_key=csem, writes=[cB])
    ctx.dma("sp", out=io[:, :], in_=dr["io"], sem_key=csem, writes=[cB])
    ctx.dma("sp", out=eb[:, :, :], in_=dr["eb"].rearrange("p (a b) -> p a b", a=4), sem_key=csem, writes=[cB])
    ctx.dma("sp", out=prm[:, :], in_=dr["prm"], sem_key=csem, writes=[cB])
    if cfg.n_odd:
        ctx.dma("sp", out=rtr[:, :], in_=dr["rtr"], sem_key=csem, writes=[cB])
    csem2 = ctx.new_sem("cst2")
    cB2 = Buf()
    ctx.dma("pool", out=cbf[:, :, :], in_=dr["cbf"].rearrange("p (a b) -> p a b", a=3), sem_key=csem2, writes=[cB2])
    ctx.op("dve", lambda e: e.memset(der[:, 80:84], EPS), reads=[cB2], writes=[cB, dB])
    ne_, no_ = cfg.n_even, cfg.n_odd
    ctx.op("dve", lambda e: e.tensor_scalar(out=der[:, 0:ne_], in0=prm[:, cols["qg"]:cols["qg"] + ne_], scalar1=0.125, scalar2=None, op0=ALU.mult),
           reads=[cB], writes=[dB])
    ctx.op("act", lambda e: e.activation(out=der[:, 8:8 + 8 * ne_], in_=prm[:, cols["snk"]:cols["snk"] + 8 * ne_], func=AF.Exp),
           reads=[cB], writes=[dB])
    if no_:
        lam = prm[:, cols["lam"]:cols["lam"] + 8 * no_]
        z = der[:, 48:48 + 8 * no_]
        c1 = der[:, 32:32 + 8 * no_]
        ctx.op("dve", lambda e: e.tensor_scalar(out=z, in0=lam, scalar1=-1.0, scalar2=None, op0=ALU.mult), reads=[cB, dB], writes=[dB])
        ctx.op("dve", lambda e: e.scalar_tensor_tensor(out=c1, in0=z, scalar=-1.0, in1=z, op0=ALU.mult, op1=ALU.max), reads=[dB], writes=[dB])
        ctx.op("act", lambda e: e.activation(out=c1, in_=c1, func=AF.Exp, scale=-1.0), reads=[dB], writes=[dB])
        ctx.op("act", lambda e: e.activation(out=c1, in_=c1, func=AF.Ln, bias=1.0), reads=[dB], writes=[dB])
        ctx.op("dve", lambda e: e.scalar_tensor_tensor(out=c1, in0=z, scalar=0.0, in1=c1, op0=ALU.max, op1=ALU.add), reads=[dB], writes=[dB])
        ctx.op("dve", lambda e: e.tensor_scalar(out=c1, in0=c1, scalar1=-8.0, scalar2=None, op0=ALU.mult), reads=[dB], writes=[dB])
        ctx.op("dve", lambda e: e.tensor_scalar(out=der[:, 64:64 + 8 * no_], in0=c1, scalar1=2.0, scalar2=None, op0=ALU.mult), reads=[dB], writes=[dB])

    def tts(tt):
        return slice(tt * 512, (tt + 1) * 512)

    def ckpt(k):
        if getattr(cfg, "stage", None) == k:
            ctx.skip = True

    def proj(wv, wbuf, c0, tt, srcs):
        b = ctx.ps_next()
        nk = len(srcs)
        for k in range(nk):
            t_, bk, kk = srcs[k]
            ctx.op("pe", lambda e: e.matmul(ps[:, b, :], lhsT=wv[:, k, c0:c0 + P], rhs=t_[:, kk, tts(tt)], start=(k == 0), stop=(k == nk - 1)),
                   reads=[wbuf, bk[tt]], writes=[pb[b]], inc=(k == nk - 1))
        return b

    hsrc = [(hT, hb, k) for k in range(KC)]
    htok = hT[:, :, :].rearrange("p c s -> p (c s)").rearrange("p (i f) -> p i f", f=D)

    with ExitStack() as ph:
        xin = [sb(ph, "xin%d" % i, [P, D], F32) for i in range(2)]
        xinb = [Buf(), Buf()]
        xsem = [ctx.new_sem(("xin", i)) for i in range(2)]
        for i in range(NB):
            k = i % 2
            ctx.dma("sp", out=xin[k][:, :], in_=dr["x"][i * P:(i + 1) * P, :], sem_key=xsem[k], writes=[xinb[k]])
            for half in range(2):
                b = ctx.ps_next()
                for c4 in range(4):
                    c = half * 4 + c4
                    ctx.op("pe", lambda e: e.transpose(out=ps[:, b, c4 * P:(c4 + 1) * P], in_=xin[k][:, c * P:(c + 1) * P], identity=cf[:, 0, :]),
                           reads=[xinb[k], cB], writes=[pb[b]], inc=(c4 == 3))
                src = ps[:, b, :].rearrange("p (c t) -> p c t", c=4)
                dst = xT[:, half * 4:(half + 1) * 4, i * P:(i + 1) * P]
                if half == 0:
                    ctx.op("act", lambda e: e.copy(out=dst, in_=src), reads=[pb[b]], writes=[xb[i // 4]])
                else:
                    ctx.op("dve", lambda e: e.tensor_copy(out=dst, in_=src), reads=[pb[b]], writes=[xb[i // 4]])
        ctx.barrier()

    def rmsnorm(gname, l, router=None):
        for tt in range(NT):
            k = flip()
            b = ctx.ps_next()
            for h2 in range(4):
                kq = flip()
                ctx.op("act", lambda e: e.activation(out=sq[:, kq, :, :], in_=xT[:, h2 * 2:(h2 + 1) * 2, tts(tt)], func=AF.Square),
                       reads=[xb[tt]], writes=[sqb[kq]])
                for c4 in range(2):
                    c = h2 * 2 + c4
                    ctx.op("pe", lambda e: e.matmul(ps[:, b, :], lhsT=cbf[:, 1, :], rhs=sq[:, kq, c4, :], start=(c == 0), stop=(c == 7)),
                           reads=[sqb[kq], cB], writes=[pb[b]], inc=(c4 == 1))
            ctx.op("act", lambda e: e.activation(out=rstd[:, k, :], in_=ps[:, b, :], func=AF.Sqrt, bias=der[:, 80:81]), reads=[pb[b], dB], writes=[rsb[k]])
            ctx.op("dve", lambda e: e.reciprocal(out=rstd[:, k, :], in_=rstd[:, k, :]), reads=[rsb[k]], writes=[rsb[k]])
            for c in range(KC if (router is None or not cfg.sparse) else 0):
                ctx.op("dve", lambda e: e.scalar_tensor_tensor(out=hT[:, c, tts(tt)], in0=xT[:, c, tts(tt)], scalar=pc(gname, l * 8 + c),
                                                               in1=rstd[:, k, :], op0=ALU.mult, op1=ALU.mult),
                       reads=[xb[tt], rsb[k], cB], writes=[hb[tt]])
            if router is not None:
                j, hf, hfb, lgb = router
                for i4 in range(4):
                    i = tt * 4 + i4
                    tsl = slice(i * P, (i + 1) * P)
                    for c in range(KC):
                        ctx.op("dve", lambda e: e.scalar_tensor_tensor(out=hf[:, c, :], in0=xT[:, c, tsl], scalar=pc(gname, l * 8 + c),
                                                                       in1=rstd[:, k, i4 * P:(i4 + 1) * P], op0=ALU.mult, op1=ALU.mult),
                               reads=[xb[tt], rsb[k], cB], writes=[hfb])
                    for c in range(KC):
                        o = (j * KC + c) * NE
                        ctx.op("pe", lambda e: e.matmul(ps[:, lgb, i * NE:(i + 1) * NE], lhsT=hf[:, c, :], rhs=rtr[:, o:o + NE], start=(c == 0), stop=(c == 7)),
                               reads=[hfb, cB], writes=[pb[lgb]], inc=(c == 7))
                    if cfg.sparse:
                        for half in range(2):
                            bT = ctx.ps_next()
                            for c4 in range(4):
                                ctx.op("pe", lambda e: e.transpose(out=ps[:, bT, c4 * P:(c4 + 1) * P], in_=hf[:, half * 4 + c4, :], identity=cf[:, 0, :]),
                                       reads=[hfb, cB], writes=[pb[bT]], inc=(c4 == 3))
                            ctx.op("act", lambda e: e.copy(out=htok[:, i, half * 512:(half + 1) * 512], in_=ps[:, bT, :]), reads=[pb[bT]], writes=[hb[tt]])

    def out_proj(wname, j, srcs):
        for op_ in range(4):
            wv, wbuf = ws.get(("cols", wname, (j,), op_ * 256, 256))
            for o2 in range(2):
                oc = op_ * 2 + o2
                for tt in range(NT):
                    b = proj(wv, wbuf, o2 * P, tt, srcs)
                    ctx.op("dve", lambda e: e.tensor_tensor(out=xT[:, oc, tts(tt)], in0=ps[:, b, :], in1=xT[:, oc, tts(tt)], op=ALU.add),
                           reads=[pb[b], xb[tt]], writes=[xb[tt]])

    def even_mixer(j, l):
        with ExitStack() as ph:
            qT = sb(ph, "qT", [P, 4, S], BF16)
            kd = sb(ph, "kd", [P, 2, S], BF16)
            va = sb(ph, "va", [P, NB, 2, 65], BF16)
            cvT = sb(ph, "cvT", [P, 4, S], BF16)
            cu = sb(ph, "cu", [P, 2, 514], F32)
            acc = sb(ph, "acc", [P, 2, 512], F32)
            E = sb(ph, "E", [P, 2, 512], F32)
            eT = sb(ph, "eT", [P, 2, 2, 512], BF16)
            den = sb(ph, "den", [P, 2, 8], F32)
            atok = sb(ph, "atok", [P, 2, 4, 64], F32)
            qb = [Buf() for _ in range(NT)]
            kb = [Buf() for _ in range(NT)]
            vb = [Buf() for _ in range(NT)]
            cvb = [Buf() for _ in range(NT)]
            cub = [Buf(), Buf()]
            accb = [Buf(), Buf()]
            Eb = Buf()
            eTb = [Buf(), Buf()]
            denb = Buf()
            atb = Buf()
            vone = Buf()
            ctx.op("dve", lambda e: e.memset(va[:, :, :, 64:65], 1.0), writes=vb)

            rmsnorm("nm", l)
            ckpt(2)

            def qknorm(b, dst, dstbuf, gcol):
                k = flip()
                ctx.op("act", lambda e: e.activation(out=sq[:, k, 0, :], in_=ps[:, b, :], func=AF.Square), reads=[pb[b]], writes=[sqb[k]])
                b2 = ctx.ps_next()
                ctx.op("pe", lambda e: e.matmul(ps[:, b2, :], lhsT=cbf[:, 2, :], rhs=sq[:, k, 0, :], start=True, stop=True),
                       reads=[sqb[k], cB], writes=[pb[b2]])
                ctx.op("act", lambda e: e.activation(out=rstd[:, k, :], in_=ps[:, b2, :], func=AF.Sqrt, bias=der[:, 80:81]), reads=[pb[b2], dB], writes=[rsb[k]])
                ctx.op("dve", lambda e: e.reciprocal(out=rstd[:, k, :], in_=rstd[:, k, :]), reads=[rsb[k]], writes=[rsb[k]])
                ctx.op("dve", lambda e: e.scalar_tensor_tensor(out=dst, in0=ps[:, b, :], scalar=gcol, in1=rstd[:, k, :], op0=ALU.mult, op1=ALU.mult),
                       reads=[pb[b], rsb[k], cB, dB], writes=[dstbuf])

            for ld in range(3):
                wv, wbuf = ws.get(("cols", "hy_w_in", (j,), ld * 256, 256))
                for o2 in range(2):
                    c = ld * 2 + o2
                    for tt in range(NT):
                        b = proj(wv, wbuf, o2 * P, tt, hsrc)
                        if c < 4:
                            qknorm(b, qT[:, c, tts(tt)], qb[tt], der[:, j:j + 1])
                        else:
                            qknorm(b, kd[:, c - 4, tts(tt)], kb[tt], pc("kg", j))
            ckpt(3)
            wv, wbuf = ws.get(("cols", "hy_w_in", (j,), 768, 128))
            for i in range(NB):
                b = ctx.ps_next()
                for k in range(KC):
                    ctx.op("pe", lambda e: e.matmul(ps[:, b, 0:P], lhsT=hT[:, k, i * P:(i + 1) * P], rhs=wv[:, k, :], start=(k == 0), stop=(k == 7)),
                           reads=[wbuf, hb[i // 4]], writes=[pb[b]], inc=(k == 7))
                ctx.op("act", lambda e: e.copy(out=va[:, i, :, 0:64], in_=ps[:, b, 0:P].rearrange("p (g d) -> p g d", g=2)),
                       reads=[pb[b]], writes=[vb[i // 4]])
            ckpt(4)
            for pcp in range(2):
                wgc, bgc = ws.get(("cols", "hy_w_in", (j,), 1408 + pcp * 256, 256))
                wu, bu_ = ws.get(("cols", "hy_w_in", (j,), 1920 + pcp * 256, 256))
                wgb, bgb = ws.get(("cols", "hy_w_in", (j,), 896 + pcp * 256, 256))
                for c2 in range(2):
                    c = pcp * 2 + c2
                    for tt in range(NT):
                        k = flip()
                        bc = proj(wgc, bgc, c2 * P, tt, hsrc)
                        bu = proj(wu, bu_, c2 * P, tt, hsrc)
                        if tt == 0:
                            ctx.op("dve", lambda e: e.memset(cu[:, k, 0:2], 0.0), writes=[cub[k]])
                        else:
                            ctx.op("dve", lambda e: e.tensor_copy(out=cu[:, k, 0:2], in_=cu[:, 1 - k, 512:514]), reads=[cub[1 - k]], writes=[cub[k]])
                        ctx.op("act", lambda e: e.copy(out=cu[:, k, 2:514], in_=ps[:, bc, :]), reads=[pb[bc]], writes=[cub[k]])
                        ctx.op("dve", lambda e: e.tensor_tensor(out=cu[:, k, 2:514], in0=ps[:, bu, :], in1=cu[:, k, 2:514], op=ALU.mult),
                               reads=[pb[bu], cub[k]], writes=[cub[k]])
                        wc = lambda kk: pc("cw", (j * 3 + kk) * 4 + c)
                        ctx.op("dve", lambda e: e.tensor_scalar(out=acc[:, k, :], in0=cu[:, k, 2:514], scalar1=wc(2), scalar2=None, op0=ALU.mult),
                               reads=[cub[k], cB], writes=[accb[k]])
                        ctx.op("dve", lambda e: e.scalar_tensor_tensor(out=acc[:, k, :], in0=cu[:, k, 1:513], scalar=wc(1), in1=acc[:, k, :], op0=ALU.mult, op1=ALU.add),
                               reads=[cub[k], accb[k], cB], writes=[accb[k]])
                        ctx.op("dve", lambda e: e.scalar_tensor_tensor(out=acc[:, k, :], in0=cu[:, k, 0:512], scalar=wc(0), in1=acc[:, k, :], op0=ALU.mult, op1=ALU.add),
                               reads=[cub[k], accb[k], cB], writes=[accb[k]])
                        bb = proj(wgb, bgb, c2 * P, tt, hsrc)
                        ctx.op("dve", lambda e: e.tensor_tensor(out=cvT[:, c, tts(tt)], in0=ps[:, bb, :], in1=acc[:, k, :], op=ALU.mult),
                               reads=[pb[bb], accb[k]], writes=[cvb[tt]])
            ckpt(5)
            esk = der[:, 8 + 8 * j:16 + 8 * j]
            for n in range(NB):
                bO = ctx.ps_next(2)
                qs = slice(n * P, (n + 1) * P)
                for g in range(2):
                    bS = ctx.ps_next(2)
                    k = flip()
                    for hh in range(4):
                        h = 4 * g + hh
                        hf_ = 0 if getattr(cfg, 'nohalf', False) else h % 2
                        rq = qT[hf_ * 64:(hf_ + 1) * 64, h // 2, qs]
                        so = (hh // 2) * 256
                        if n > 0:
                            ctx.op("pe", lambda e: e.matmul(ps[:, bS + hh % 2, so:so + P], lhsT=kd[hf_ * 64:(hf_ + 1) * 64, g, (n - 1) * P:n * P], rhs=rq, start=True, stop=True),
                                   reads=[kb[(n - 1) // 4], qb[n // 4]], writes=[pb[bS + hh % 2]], inc=False)
                        ctx.op("pe", lambda e: e.matmul(ps[:, bS + hh % 2, so + P:so + 2 * P], lhsT=kd[hf_ * 64:(hf_ + 1) * 64, g, qs], rhs=rq, start=True, stop=True),
                               reads=[kb[n // 4], qb[n // 4]], writes=[pb[bS + hh % 2]], inc=(hh == 3))
                    for b2_ in range(2):
                        ctx.op("act", lambda e: e.activation(out=E[:, b2_, :], in_=ps[:, bS + b2_, :], func=AF.Exp), reads=[pb[bS + b2_]], writes=[Eb])
                    ctx.op("dve", lambda e: e.tensor_tensor(out=eT[:, k, :, :], in0=E[:, :, :], in1=eb[:, 2 * g:2 * g + 2, :], op=ALU.mult),
                           reads=[Eb, cB], writes=[eTb[k]])
                    ckpt(51)
                    for hh in range(4):
                        h = 4 * g + hh
                        so = (hh // 2) * 256
                        oh = ps[:, bO + h // 4, (h % 4) * P:(h % 4) * P + 65]
                        if n > 0:
                            ctx.op("pe", lambda e: e.matmul(oh, lhsT=eT[:, k, hh % 2, so:so + P], rhs=va[:, n - 1, g, :], start=True, stop=False),
                                   reads=[eTb[k], vb[(n - 1) // 4]], writes=[pb[bO + h // 4]], inc=False)
                        ctx.op("pe", lambda e: e.matmul(oh, lhsT=eT[:, k, hh % 2, so + P:so + 2 * P], rhs=va[:, n, g, :], start=(n == 0), stop=True),
                               reads=[eTb[k], vb[n // 4]], writes=[pb[bO + h // 4]], inc=(hh == 3))
                ckpt(52)
                Ov = ps[:, bO:bO + 2, :].rearrange("p b (h d) -> p b h d", h=4)
                dn = den[:, 0, :].rearrange("p (b h) -> p b h", b=2)
                rc = den[:, 1, :].rearrange("p (b h) -> p b h", b=2)
                ctx.op("dve", lambda e: e.tensor_tensor(out=dn, in0=Ov[:, :, :, 64], in1=esk.rearrange("p (b h) -> p b h", b=2), op=ALU.add),
                       reads=[pb[bO], pb[bO + 1], dB], writes=[denb])
                ctx.op("dve", lambda e: e.reciprocal(out=den[:, 1, :], in_=den[:, 0, :]), reads=[denb], writes=[denb])
                ctx.op("dve", lambda e: e.tensor_tensor(out=atok[:, :, :, :], in0=Ov[:, :, :, 0:64], in1=rc.unsqueeze(3).broadcast_to([P, 2, 4, 64]), op=ALU.mult),
                       reads=[pb[bO], pb[bO + 1], denb], writes=[atb])
                ckpt(53)
                bT = ctx.ps_next()
                a2 = atok[:, :, :, :].rearrange("p b h d -> p (b h d)")
                for c in range(4):
                    ctx.op("pe", lambda e: e.transpose(out=ps[:, bT, c * P:(c + 1) * P], in_=a2[:, c * P:(c + 1) * P], identity=cf[:, 0, :]),
                           reads=[atb, cB], writes=[pb[bT]], inc=(c == 3))
                ctx.op("act", lambda e: e.copy(out=hT[:, 0:4, qs], in_=ps[:, bT, :].rearrange("p (c t) -> p c t", c=4)),
                       reads=[pb[bT]], writes=[hb[n // 4]])
            ckpt(6)
            out_proj("hy_w_out", j, [(hT, hb, k) for k in range(4)] + [(cvT, cvb, k) for k in range(4)])
            ctx.barrier()

    def odd_mixer(j, l):
        with ExitStack() as ph:
            mixT = sb(ph, "mixT", [P, KC, S], BF16)
            gw = sb(ph, "gw", [P, 2, 8, P], BF16)
            xbuf = sb(ph, "xbuf", [P, 2, 515], F32)
            tmp = sb(ph, "tmp", [P, 9, 512], F32)
            xcb = sb(ph, "xcb", [P, 512], BF16)
            hs = sb(ph, "hs", [P, 2, 512], F32)
            mb = [Buf() for _ in range(NT)]
            gwb = Buf()
            xbb = [Buf(), Buf()]
            tb = [Buf() for _ in range(9)]
            xcbb = Buf()
            hsb = [Buf(), Buf()]
            gsem = ctx.new_sem(("gw", l))
            ctx.dma("pool", out=gw[:, 0, :, :], in_=dr["rg_ga_%d" % j].rearrange("h i j -> i h j"), sem_key=gsem, writes=[gwb])
            ctx.dma("pool", out=gw[:, 1, :, :], in_=dr["rg_gx_%d" % j].rearrange("h i j -> i h j"), sem_key=gsem, writes=[gwb])
            rmsnorm("nm", l)
            T1, SG, YG, XC, R, IG, A, TT_, BB = range(9)
            for c in range(KC):
                wv, wbuf = ws.get(("cols", "rg_w_in", (j,), c * 256, 256))
                for tt in range(NT):
                    k = flip()
                    by = proj(wv, wbuf, 0, tt, hsrc)
                    bx = proj(wv, wbuf, P, tt, hsrc)
                    py, px = ps[:, by, :], ps[:, bx, :]
                    t = lambda i: tmp[:, i, :]
                    ctx.op("act", lambda e: e.activation(out=t(T1), in_=py, func=AF.Square), reads=[pb[by]], writes=[tb[T1]])
                    ctx.op("dve", lambda e: e.tensor_scalar(out=t(T1), in0=t(T1), scalar1=0.044715, scalar2=1.0, op0=ALU.mult, op1=ALU.add),
                           reads=[tb[T1]], writes=[tb[T1]])
                    ctx.op("dve", lambda e: e.tensor_tensor(out=t(T1), in0=py, in1=t(T1), op=ALU.mult), reads=[pb[by], tb[T1]], writes=[tb[T1]])
                    ctx.op("act", lambda e: e.activation(out=t(SG), in_=t(T1), func=AF.Sigmoid, scale=1.5957691216057308), reads=[tb[T1]], writes=[tb[SG]])
                    ctx.op("dve", lambda e: e.tensor_tensor(out=t(YG), in0=py, in1=t(SG), op=ALU.mult), reads=[pb[by], tb[SG]], writes=[tb[YG]])
                    if tt == 0:
                        ctx.op("dve", lambda e: e.memset(xbuf[:, k, 0:3], 0.0), writes=[xbb[k]])
                    else:
                        ctx.op("dve", lambda e: e.tensor_copy(out=xbuf[:, k, 0:3], in_=xbuf[:, 1 - k, 512:515]), reads=[xbb[1 - k]], writes=[xbb[k]])
                    ctx.op("act", lambda e: e.copy(out=xbuf[:, k, 3:515], in_=px), reads=[pb[bx]], writes=[xbb[k]])
                    wc = lambda kk: pc("rcw", (j * 4 + kk) * 8 + c)
                    ctx.op("dve", lambda e: e.tensor_scalar(out=t(XC), in0=xbuf[:, k, 3:515], scalar1=wc(3), scalar2=pc("rcb", j * 8 + c), op0=ALU.mult, op1=ALU.add),
                           reads=[xbb[k], cB], writes=[tb[XC]])
                    for kk in (2, 1, 0):
                        ctx.op("dve", lambda e: e.scalar_tensor_tensor(out=t(XC), in0=xbuf[:, k, kk:kk + 512], scalar=wc(kk), in1=t(XC), op0=ALU.mult, op1=ALU.add),
                               reads=[xbb[k], tb[XC], cB], writes=[tb[XC]])
                    ctx.op("act", lambda e: e.copy(out=xcb[:, :], in_=t(XC)), reads=[tb[XC]], writes=[xcbb])
                    br = ctx.ps_next()
                    ctx.op("pe", lambda e: e.matmul(ps[:, br, :], lhsT=gw[:, 0, c, :], rhs=xcb[:, :], start=True, stop=True), reads=[gwb, xcbb], writes=[pb[br]])
                    bi = ctx.ps_next()
                    ctx.op("pe", lambda e: e.matmul(ps[:, bi, :], lhsT=gw[:, 1, c, :], rhs=xcb[:, :], start=True, stop=True), reads=[gwb, xcbb], writes=[pb[bi]])
                    ctx.op("act", lambda e: e.activation(out=t(R), in_=ps[:, br, :], func=AF.Sigmoid, bias=pc("gab", j * 8 + c)), reads=[pb[br], cB], writes=[tb[R]])
                    ctx.op("act", lambda e: e.activation(out=t(IG), in_=ps[:, bi, :], func=AF.Sigmoid, bias=pc("gxb", j * 8 + c)), reads=[pb[bi], cB], writes=[tb[IG]])
                    ctx.op("act", lambda e: e.activation(out=t(A), in_=t(R), func=AF.Exp, scale=der[:, 32 + j * 8 + c:33 + j * 8 + c]), reads=[tb[R], dB], writes=[tb[A]])
                    ctx.op("act", lambda e: e.activation(out=t(TT_), in_=t(R), func=AF.Exp, scale=der[:, 64 + j * 8 + c:65 + j * 8 + c]), reads=[tb[R], dB], writes=[tb[TT_]])
                    ctx.op("act", lambda e: e.activation(out=t(TT_), in_=t(TT_), func=AF.Relu, scale=-1.0, bias=1.0), reads=[tb[TT_]], writes=[tb[TT_]])
                    ctx.op("act", lambda e: e.activation(out=t(TT_), in_=t(TT_), func=AF.Sqrt), reads=[tb[TT_]], writes=[tb[TT_]])
                    if tt == 0:
                        ctx.op("dve", lambda e: e.memset(tmp[:, TT_, 0:1], 1.0), reads=[tb[TT_]], writes=[tb[TT_]])
                    ctx.op("dve", lambda e: e.tensor_tensor(out=t(BB), in0=t(IG), in1=t(XC), op=ALU.mult), reads=[tb[IG], tb[XC]], writes=[tb[BB]])
                    ctx.op("dve", lambda e: e.tensor_tensor(out=t(BB), in0=t(BB), in1=t(TT_), op=ALU.mult), reads=[tb[BB], tb[TT_]], writes=[tb[BB]])
                    init = 0.0 if tt == 0 else hs[:, 1 - k, 511:512]
                    ctx.op("dve", lambda e: e.tensor_tensor_scan(out=hs[:, k, :], data0=t(A), data1=t(BB), initial=init, op0=ALU.mult, op1=ALU.add),
                           reads=[tb[A], tb[BB], hsb[1 - k]], writes=[hsb[k]])
                    ctx.op("dve", lambda e: e.tensor_tensor(out=mixT[:, c, tts(tt)], in0=t(YG), in1=hs[:, k, :], op=ALU.mult),
                           reads=[tb[YG], hsb[k]], writes=[mb[tt]])
            out_proj("rg_w_out", j, [(mixT, mb, k) for k in range(KC)])
            ctx.barrier()

    actpar = [0]

    dpend = [None]

    def dense_down(ka, wd, bwd, actb, actB):
        for oc in range(KC):
            for tt in range(NT):
                b = ctx.ps_next()
                for f in range(FG):
                    ctx.op("pe", lambda e: e.matmul(ps[:, b, :], lhsT=wd[:, f, oc * P:(oc + 1) * P], rhs=actb[:, ka, f, tts(tt)], start=(f == 0), stop=(f == FG - 1)),
                           reads=[bwd, actB[ka][tt]], writes=[pb[b]], inc=(f == FG - 1))
                ctx.op("dve", lambda e: e.tensor_tensor(out=xT[:, oc, tts(tt)], in0=ps[:, b, :], in1=xT[:, oc, tts(tt)], op=ALU.add),
                       reads=[pb[b], xb[tt]], writes=[xb[tt]])

    def swiglu(names, pre, loc, G=None, Gb=None):
        actb, actB, sg, sgb, tm, tmb = loc
        for g in range(NF // FG):
            wg, bwg = ws.get(("cols", names[0], pre, g * FG * P, FG * P))
            wu, bwu = ws.get(("cols", names[1], pre, g * FG * P, FG * P))
            wd, bwd = ws.get(("rows", names[2], pre, g * FG * P, FG))
            actpar[0] ^= 1
            ka = actpar[0]
            for f in range(FG):
                for tt in range(NT):
                    k = flip()
                    bg = proj(wg, bwg, f * P, tt, hsrc)
                    bu = proj(wu, bwu, f * P, tt, hsrc)
                    ctx.op("act", lambda e: e.activation(out=sg[:, k, :], in_=ps[:, bg, :], func=AF.Silu), reads=[pb[bg]], writes=[sgb[k]])
                    if G is None:
                        ctx.op("dve", lambda e: e.tensor_tensor(out=actb[:, ka, f, tts(tt)], in0=ps[:, bu, :], in1=sg[:, k, :], op=ALU.mult),
                               reads=[pb[bu], sgb[k]], writes=[actB[ka][tt]])
                    else:
                        ctx.op("dve", lambda e: e.tensor_tensor(out=tm[:, k, :], in0=ps[:, bu, :], in1=sg[:, k, :], op=ALU.mult),
                               reads=[pb[bu], sgb[k]], writes=[tmb[k]])
                        ctx.op("dve", lambda e: e.tensor_tensor(out=actb[:, ka, f, tts(tt)], in0=tm[:, k, :], in1=G[:, tts(tt)], op=ALU.mult),
                               reads=[tmb[k], Gb], writes=[actB[ka][tt]])
            if dpend[0] is not None:
                dense_down(*dpend[0])
            dpend[0] = (ka, wd, bwd, actb, actB)
        dense_down(*dpend[0])
        dpend[0] = None

    def ffn_locals(ph):
        actb = sb(ph, "actb", [P, 2, FG, S], BF16)
        actB = [[Buf() for _ in range(NT)] for _ in range(2)]
        sg = sb(ph, "sg", [P, 2, 512], F32)
        tm = sb(ph, "tm", [P, 2, 512], F32)
        return actb, actB, sg, [Buf(), Buf()], tm, [Buf(), Buf()]

    def dense_ffn(j, l):
        with ExitStack() as ph:
            loc = ffn_locals(ph)
            rmsnorm("nf", l)
            swiglu(("ffn_w_gate", "ffn_w_up", "ffn_w_down"), (j,), loc)
            ctx.barrier()

    def moe_sparse(j, l):
        CAP = cfg.CAP
        NBK = CAP // P
        HB = NB // 2
        HS = S // 2
        with ExitStack() as ph:
            hf = sb(ph, "hf", [P, KC, P], F32)
            hfb = Buf()
            rt = sb(ph, "rt", [P, 8, NB, NE], F32)
            rs_ = sb(ph, "rs_", [P, 6, NB], F32)
            cno = sb(ph, "cno", [P, 2, NB, NE], F32)
            rb = Buf()
            Dg = hf[:, 0:HB, :]
            Dgb = hfb
            rows = sb(ph, "rows", [P, 2, HS], F32)
            rowb = [Buf(), Buf()]
            hg = sb(ph, "hg", [P, 2, KC, CAP], BF16)
            hgb = [Buf(), Buf()]
            yacc = sb(ph, "yacc", [P, 2, NBK, D], F32)
            yaccB = [[Buf() for _ in range(NBK)] for _ in range(2)]
            ybf = hg[:, 0, :, :].rearrange("p k c -> p (k c)").rearrange("p (b d) -> p b d", d=D)
            ybfB = hgb[0]
            actb = sb(ph, "actb", [P, 2, FG, CAP], BF16)
            actB = [Buf(), Buf()]
            sg = sb(ph, "sg", [P, 2, CAP], F32)
            sgb = [Buf(), Buf()]
            Sel = sb(ph, "Sel", [P, 2, CAP], BF16)
            selb = [Buf(), Buf()]
            SelT = sb(ph, "SelT", [P, 1, NBK, 512], BF16)
            stb = [Buf()]
            lgb = ctx.ps_next()
            ctx.reserved.add(lgb)
            rmsnorm("nf", l, router=(j, hf, hfb, lgb))
            L, EQ1, L2, EQ2, GT, TMP, MASK, POS = (rt[:, i, :, :] for i in range(8))
            M1, M2, DD, ED, G1, G2 = (rs_[:, i, :] for i in range(6))
            CNT, OFF = cno[:, 0, :, :], cno[:, 1, :, :]
            bc = lambda a: a.unsqueeze(2).broadcast_to([P, NB, NE])
            flat = lambda a: a.rearrange("p a b -> p (a b)")
            v3 = lambda a: a.rearrange("p (a b) -> p a b", b=NE)
            ctx.op("dve", lambda e: e.tensor_copy(out=L, in_=v3(ps[:, lgb, 0:NB * NE])), reads=[pb[lgb]], writes=[rb])
            ctx.reserved.discard(lgb)
            for fn in (
                lambda e: e.tensor_reduce(out=M1, in_=L, axis=AX.X, op=ALU.max),
                lambda e: e.tensor_tensor(out=EQ1, in0=L, in1=bc(M1), op=ALU.is_equal),
                lambda e: e.scalar_tensor_tensor(out=L2, in0=EQ1, scalar=-1e30, in1=L, op0=ALU.mult, op1=ALU.add),
                lambda e: e.tensor_reduce(out=M2, in_=L2, axis=AX.X, op=ALU.max),
                lambda e: e.tensor_tensor(out=EQ2, in0=L2, in1=bc(M2), op=ALU.is_equal),
                lambda e: e.tensor_tensor(out=DD, in0=M2, in1=M1, op=ALU.subtract),
            ):
                ctx.op("dve", fn, reads=[rb], writes=[rb])
            ctx.op("act", lambda e: e.activation(out=ED, in_=DD, func=AF.Exp), reads=[rb], writes=[rb])
            for fn in (
                lambda e: e.tensor_scalar(out=G2, in0=ED, scalar1=1.0, scalar2=None, op0=ALU.add),
                lambda e: e.reciprocal(out=G1, in_=G2),
                lambda e: e.tensor_tensor(out=G2, in0=ED, in1=G1, op=ALU.mult),
                lambda e: e.tensor_tensor(out=GT, in0=EQ1, in1=bc(G1), op=ALU.mult),
                lambda e: e.tensor_tensor(out=TMP, in0=EQ2, in1=bc(G2), op=ALU.mult),
                lambda e: e.tensor_tensor(out=GT, in0=GT, in1=TMP, op=ALU.add),
                lambda e: e.tensor_tensor(out=MASK, in0=EQ1, in1=EQ2, op=ALU.add),
            ):
                ctx.op("dve", fn, reads=[rb], writes=[rb])
            b1 = ctx.ps_next()
            ctx.op("pe", lambda e: e.matmul(ps[:, b1, 0:NB * NE], lhsT=cf[:, 2, :], rhs=flat(MASK), start=True, stop=True), reads=[rb, cB], writes=[pb[b1]])
            b2 = ctx.ps_next()
            ctx.op("pe", lambda e: e.matmul(ps[:, b2, 0:NB * NE], lhsT=cf[:, 1, :], rhs=flat(MASK), start=True, stop=True), reads=[rb, cB], writes=[pb[b2]])
            ctx.op("dve", lambda e: e.tensor_copy(out=CNT, in_=v3(ps[:, b2, 0:NB * NE])), reads=[pb[b2]], writes=[rb])
            ctx.op("dve", lambda e: e.memset(cno[:, 1, :, :], 0.0), reads=[rb], writes=[rb])
            for i in range(NB):
                if i % HB:
                    ctx.op("dve", lambda e: e.tensor_tensor(out=cno[:, 1, i, :], in0=cno[:, 1, i - 1, :], in1=cno[:, 0, i - 1, :], op=ALU.add), reads=[rb], writes=[rb])
            ctx.op("dve", lambda e: e.tensor_tensor(out=POS, in0=v3(ps[:, b1, 0:NB * NE]), in1=OFF, op=ALU.add), reads=[pb[b1], rb], writes=[rb])

            def gather(ex, hp):
                tb0 = hp * HB
                for ch in range(2):
                    banks = [ctx.ps_next() for _ in range(4)]
                    for blk in range(HB):
                        k = flip()
                        ctx.op("dve", lambda e: e.tensor_scalar(out=Sel[:, k, :], in0=io[:, 0:CAP], scalar1=rt[:, 7, tb0 + blk, ex:ex + 1],
                                                                scalar2=rt[:, 6, tb0 + blk, ex:ex + 1], op0=ALU.is_equal, op1=ALU.mult),
                               reads=[rb, cB], writes=[selb[k]])
                        for c in range(4):
                            ctx.op("pe", lambda e: e.matmul(ps[:, banks[c], 0:CAP], lhsT=htok[:, tb0 + blk, (4 * ch + c) * P:(4 * ch + c + 1) * P], rhs=Sel[:, k, :],
                                                            start=(blk == 0), stop=(blk == HB - 1)),
                                   reads=[hb[(tb0 + blk) // 4], selb[k]], writes=[pb[banks[c]]], inc=(c == 3))
                    for c in range(4):
                        ctx.op("act", lambda e: e.copy(out=hg[:, hp, 4 * ch + c, :], in_=ps[:, banks[c], 0:CAP]), reads=[pb[banks[c]]], writes=[hgb[hp]])

            def rows_build(ex, hp):
                tb0 = hp * HB
                for ri, val in ((0, GT), (1, POS)):
                    ctx.op("dve", lambda e: e.tensor_tensor(out=Dg, in0=cf[:, 0:1, :].broadcast_to([P, HB, P]),
                                                            in1=val[:, tb0:tb0 + HB, ex:ex + 1].broadcast_to([P, HB, P]), op=ALU.mult),
                           reads=[rb, cB], writes=[Dgb])
                    for q in range(HS // 512):
                        b = ctx.ps_next()
                        ctx.op("pe", lambda e: e.matmul(ps[:, b, :], lhsT=cf[:, 1, :], rhs=Dg[:, q * 4:(q + 1) * 4, :].rearrange("p a b -> p (a b)"), start=True, stop=True),
                               reads=[Dgb, cB], writes=[pb[b]])
                        ctx.op("act", lambda e: e.copy(out=rows[:, ri, q * 512:(q + 1) * 512], in_=ps[:, b, :]), reads=[pb[b]], writes=[rowb[ri]])

            def scatter(ex, hp):
                for blk in range(NBK):
                    ctx.op("act", lambda e: e.copy(out=ybf[:, blk, :], in_=yacc[:, hp, blk, :]), reads=[yaccB[hp][blk]], writes=[ybfB])
                for t2 in range(HS // 512):
                    k = 0
                    tsl = slice(t2 * 512, (t2 + 1) * 512)
                    for blk in range(NBK):
                        ctx.op("dve", lambda e: e.scalar_tensor_tensor(out=SelT[:, k, blk, :], in0=rows[:, 1, tsl], scalar=io[:, CAP + blk:CAP + blk + 1],
                                                                       in1=rows[:, 0, tsl], op0=ALU.is_equal, op1=ALU.mult),
                               reads=[rowb[0], rowb[1], cB], writes=[stb[k]])
                    tt = hp * (NT // 2) + t2
                    for c in range(KC):
                        b = ctx.ps_next()
                        for blk in range(NBK):
                            ctx.op("pe", lambda e: e.matmul(ps[:, b, :], lhsT=ybf[:, blk, c * P:(c + 1) * P], rhs=SelT[:, k, blk, :], start=(blk == 0), stop=(blk == NBK - 1)),
                                   reads=[ybfB, stb[k]], writes=[pb[b]], inc=(blk == NBK - 1))
                        ctx.op("dve", lambda e: e.tensor_tensor(out=xT[:, c, tts(tt)], in0=ps[:, b, :], in1=xT[:, c, tts(tt)], op=ALU.add),
                               reads=[pb[b], xb[tt]], writes=[xb[tt]])

            def ffn(ex):
                def emit_down(g, hp, ka, wd, bwd):
                    for blk in range(NBK):
                        for hh in range(2):
                            b = ctx.ps_next()
                            for f in range(FG):
                                ctx.op("pe", lambda e: e.matmul(ps[:, b, :], lhsT=actb[:, ka, f, blk * P:(blk + 1) * P], rhs=wd[:, f, hh * 512:(hh + 1) * 512],
                                                                start=(f == 0), stop=(f == FG - 1)),
                                       reads=[bwd, actB[ka]], writes=[pb[b]], inc=(f == FG - 1))
                            ya = yacc[:, hp, blk, hh * 512:(hh + 1) * 512]
                            if g == 0:
                                ctx.op("act", lambda e: e.copy(out=ya, in_=ps[:, b, :]), reads=[pb[b]], writes=[yaccB[hp][blk]])
                            else:
                                ctx.op("dve", lambda e: e.tensor_tensor(out=ya, in0=ps[:, b, :], in1=ya, op=ALU.add), reads=[pb[b], yaccB[hp][blk]], writes=[yaccB[hp][blk]])

                pend = [None]
                for g in range(NF // FG):
                    wg, bwg = ws.get(("cols", "moe_w_gate", (j, ex), g * FG * P, FG * P))
                    wu, bwu = ws.get(("cols", "moe_w_up", (j, ex), g * FG * P, FG * P))
                    wd, bwd = ws.get(("rows", "moe_w_down", (j, ex), g * FG * P, FG))
                    for hp in range(2):
                        actpar[0] ^= 1
                        ka = actpar[0]
                        for f in range(FG):
                            k = flip()
                            bg = ctx.ps_next()
                            for kk in range(KC):
                                ctx.op("pe", lambda e: e.matmul(ps[:, bg, 0:CAP], lhsT=wg[:, kk, f * P:(f + 1) * P], rhs=hg[:, hp, kk, :], start=(kk == 0), stop=(kk == 7)),
                                       reads=[bwg, hgb[hp]], writes=[pb[bg]], inc=(kk == 7))
                            bu = ctx.ps_next()
                            for kk in range(KC):
                                ctx.op("pe", lambda e: e.matmul(ps[:, bu, 0:CAP], lhsT=wu[:, kk, f * P:(f + 1) * P], rhs=hg[:, hp, kk, :], start=(kk == 0), stop=(kk == 7)),
                                       reads=[bwu, hgb[hp]], writes=[pb[bu]], inc=(kk == 7))
                            ctx.op("act", lambda e: e.activation(out=sg[:, k, :], in_=ps[:, bg, 0:CAP], func=AF.Silu), reads=[pb[bg]], writes=[sgb[k]])
                            ctx.op("dve", lambda e: e.tensor_tensor(out=actb[:, ka, f, :], in0=ps[:, bu, 0:CAP], in1=sg[:, k, :], op=ALU.mult),
                                   reads=[pb[bu], sgb[k]], writes=[actB[ka]])
                        if pend[0] is not None:
                            emit_down(*pend[0])
                        pend[0] = (g, hp, ka, wd, bwd)
                rows_build(ex, 0)
                emit_down(*pend[0])
                pend[0] = None

            gather(0, 0)
            gather(0, 1)
            for ex in range(NE):
                ffn(ex)
                if ex + 1 < NE:
                    gather(ex + 1, 1)
                scatter(ex, 0)
                rows_build(ex, 1)
                scatter(ex, 1)
                if ex + 1 < NE:
                    gather(ex + 1, 0)
            ctx.barrier()

    def moe(j, l):
        with ExitStack() as ph:
            loc = ffn_locals(ph)
            hf = sb(ph, "hf", [P, KC, P], F32)
            hfb = Buf()
            Gt = sb(ph, "Gt", [P, 2, S], F32)
            Gb = [Buf(), Buf()]
            Dg = sb(ph, "Dg", [P, NB, P], F32)
            Dgb = Buf()
            rt = sb(ph, "rt", [P, 6, NB, NE], F32)
            rs_ = sb(ph, "rs_", [P, 6, NB], F32)
            rb = Buf()
            lgb = ctx.ps_next()
            ctx.reserved.add(lgb)
            rmsnorm("nf", l, router=(j, hf, hfb, lgb))
            L, EQ1, L2, EQ2, GT, TMP = (rt[:, i, :, :] for i in range(6))
            M1, M2, DD, ED, G1, G2 = (rs_[:, i, :] for i in range(6))
            bc = lambda a: a.unsqueeze(2).broadcast_to([P, NB, NE])
            ctx.op("dve", lambda e: e.tensor_copy(out=L, in_=ps[:, lgb, 0:NB * NE].rearrange("p (a b) -> p a b", b=NE)), reads=[pb[lgb]], writes=[rb])
            ctx.reserved.discard(lgb)
            seq = [
                lambda e: e.tensor_reduce(out=M1, in_=L, axis=AX.X, op=ALU.max),
                lambda e: e.tensor_tensor(out=EQ1, in0=L, in1=bc(M1), op=ALU.is_equal),
                lambda e: e.scalar_tensor_tensor(out=L2, in0=EQ1, scalar=-1e30, in1=L, op0=ALU.mult, op1=ALU.add),
                lambda e: e.tensor_reduce(out=M2, in_=L2, axis=AX.X, op=ALU.max),
                lambda e: e.tensor_tensor(out=EQ2, in0=L2, in1=bc(M2), op=ALU.is_equal),
                lambda e: e.tensor_tensor(out=DD, in0=M2, in1=M1, op=ALU.subtract),
            ]
            for fn in seq:
                ctx.op("dve", fn, reads=[rb], writes=[rb])
            ctx.op("act", lambda e: e.activation(out=ED, in_=DD, func=AF.Exp), reads=[rb], writes=[rb])
            seq = [
                lambda e: e.tensor_scalar(out=G2, in0=ED, scalar1=1.0, scalar2=None, op0=ALU.add),
                lambda e: e.reciprocal(out=G1, in_=G2),
                lambda e: e.tensor_tensor(out=G2, in0=ED, in1=G1, op=ALU.mult),
                lambda e: e.tensor_tensor(out=GT, in0=EQ1, in1=bc(G1), op=ALU.mult),
                lambda e: e.tensor_tensor(out=TMP, in0=EQ2, in1=bc(G2), op=ALU.mult),
                lambda e: e.tensor_tensor(out=GT, in0=GT, in1=TMP, op=ALU.add),
            ]
            for fn in seq:
                ctx.op("dve", fn, reads=[rb], writes=[rb])
            for ex in range(NE):
                ke = ex % 2
                ctx.op("dve", lambda e: e.tensor_tensor(out=Dg[:, :, :], in0=cf[:, 0:1, :].broadcast_to([P, NB, P]),
                                                        in1=rt[:, 4, :, ex:ex + 1].broadcast_to([P, NB, P]), op=ALU.mult),
                       reads=[rb, cB], writes=[Dgb])
                for tt in range(NT):
                    b = ctx.ps_next()
                    ctx.op("pe", lambda e: e.matmul(ps[:, b, :], lhsT=cf[:, 1, :], rhs=Dg[:, tt * 4:(tt + 1) * 4, :].rearrange("p a b -> p (a b)"), start=True, stop=True),
                           reads=[Dgb, cB], writes=[pb[b]])
                    ctx.op("act", lambda e: e.copy(out=Gt[:, ke, tts(tt)], in_=ps[:, b, :]), reads=[pb[b]], writes=[Gb[ke]])
                swiglu(("moe_w_gate", "moe_w_up", "moe_w_down"), (j, ex), loc, G=Gt[:, ke, :], Gb=Gb[ke])
            ctx.barrier()

    try:
        ckpt(1)
        for l in layers:
            j = l // 2
            only = getattr(cfg, "only", None)
            if l % 2 == 0:
                if only in (None, "mixer"):
                    even_mixer(j, l)
                if only in (None, "ffn"):
                    dense_ffn(j, l)
            else:
                if only in (None, "mixer"):
                    odd_mixer(j, l)
                if only in (None, "ffn"):
                    (moe_sparse if cfg.sparse else moe)(j, l)
    except StopEmit:
        pass
    if ctx.skip:
        ctx.skip = False
        ctx.barrier()

    with ExitStack() as ph:
        xo = [sb(ph, "xo%d" % i, [P, D], F32) for i in range(2)]
        xob = [Buf(), Buf()]
        osem = [ctx.new_sem(("xo", i)) for i in range(2)]
        for i in range(NB):
            k = i % 2
            for half in range(2):
                b = ctx.ps_next()
                for c4 in range(4):
                    c = half * 4 + c4
                    ctx.op("pe", lambda e: e.transpose(out=ps[:, b, c4 * P:(c4 + 1) * P], in_=xT[:, c, i * P:(i + 1) * P], identity=cf[:, 0, :]),
                           reads=[xb[i // 4], cB], writes=[pb[b]], inc=(c4 == 3))
                if half == 0:
                    ctx.op("act", lambda e: e.copy(out=xo[k][:, 0:512], in_=ps[:, b, :]), reads=[pb[b]], writes=[xob[k]])
                else:
                    ctx.op("dve", lambda e: e.tensor_copy(out=xo[k][:, 512:1024], in_=ps[:, b, :]), reads=[pb[b]], writes=[xob[k]])
            ctx.dma("sp", out=out_d[i * P:(i + 1) * P, :], in_=xo[k][:, :], sem_key=osem[k], reads=[xob[k]])
        if not ctx.dry:
            for kk in osem:
                nc.sync.wait_ge(ctx.sem[kk], ctx.cnt[kk])
    es.close()
    return ws.rec


def build(cfg, layers):
    nc0 = bass.Bass("TRN2", target_bir_lowering=False)
    specs = emit(nc0, Ctx(nc0, True), cfg, layers, None)
    nc = bass.Bass("TRN2", target_bir_lowering=False)
    ctx = Ctx(nc, False)
    emit(nc, ctx, cfg, layers, specs)
    return nc, ctx


def kernel(**inputs):
    cfg = Cfg()
    x = np.asarray(inputs["x"], np.float32)
    sh = prep_shared(cfg, inputs)
    nc, _ = build(cfg, list(range(cfg.depth)))
    in_maps = []
    for b in range(8):
        m = dict(sh)
        m["x"] = np.ascontiguousarray(x[b])
        in_maps.append(m)
    res = run_bass_kernel_spmd(nc, in_maps, core_ids=list(range(8)))
    return np.stack([np.asarray(r["out"], np.float32) for r in res.results], axis=0)
```

```python
import numpy as np
from contextlib import ExitStack
import concourse.bass as bass
import concourse.mybir as mybir
from concourse.bass_utils import run_bass_kernel_spmd

F32 = mybir.dt.float32
BF16 = mybir.dt.bfloat16
AF = mybir.ActivationFunctionType
ALU = mybir.AluOpType
AX = mybir.AxisListType
P = 128
D = 1024
KC = 8
EPS = 1e-6
SLOT = 2048


class Cfg:
    def __init__(self, S=2048, DFF=3584, NE=8, depth=4, FG=2, nslots=6):
        self.S, self.DFF, self.NE, self.depth, self.FG, self.nslots = S, DFF, NE, depth, FG, nslots
        self.n_even = (depth + 1) // 2
        self.n_odd = depth // 2
        self.sparse = True
        self.CAP = 384


def param_layout(cfg):
    cols = {}
    o = 0
    for name, w in (("nm", cfg.depth * 8), ("nf", cfg.depth * 8), ("qg", cfg.n_even), ("kg", cfg.n_even),
                    ("cw", cfg.n_even * 12), ("rcw", cfg.n_odd * 32), ("rcb", cfg.n_odd * 8),
                    ("gab", cfg.n_odd * 8), ("gxb", cfg.n_odd * 8), ("lam", cfg.n_odd * 8),
                    ("snk", cfg.n_even * 8)):
        cols[name] = o
        o += w
    return cols, o


def prep_shared(cfg, inp):
    f = np.float32
    cols, npar = param_layout(cfg)
    prm = np.zeros((P, npar), f)

    def fm(v):
        v = np.asarray(v, f)
        C = v.shape[-1] // P
        return np.moveaxis(v.reshape(v.shape[:-1] + (C, P)), -1, 0)

    prm[:, cols["nm"]:cols["nm"] + cfg.depth * 8] = fm(inp["norm_mix"]).reshape(P, -1)
    prm[:, cols["nf"]:cols["nf"] + cfg.depth * 8] = fm(inp["norm_ffn"]).reshape(P, -1)
    prm[:, cols["qg"]:cols["qg"] + cfg.n_even] = np.tile(np.asarray(inp["hy_q_gain"], f).T, (2, 1))
    prm[:, cols["kg"]:cols["kg"] + cfg.n_even] = np.tile(np.asarray(inp["hy_k_gain"], f).T, (2, 1))
    prm[:, cols["cw"]:cols["cw"] + cfg.n_even * 12] = fm(inp["hy_conv_w"]).reshape(P, -1)
    if cfg.n_odd:
        prm[:, cols["rcw"]:cols["rcw"] + cfg.n_odd * 32] = fm(inp["rg_conv_w"]).reshape(P, -1)
        for nm_, key in (("rcb", "rg_conv_b"), ("gab", "rg_gate_a_b"), ("gxb", "rg_gate_x_b"), ("lam", "rg_lambda")):
            prm[:, cols[nm_]:cols[nm_] + cfg.n_odd * 8] = fm(inp[key]).reshape(P, -1)
    prm[:, cols["snk"]:cols["snk"] + cfg.n_even * 8] = np.broadcast_to(
        np.asarray(inp["hy_sinks"], f).reshape(1, -1), (P, cfg.n_even * 8))

    cbf = np.zeros((P, 3, P), f)
    cbf[:, 0] = np.eye(P, dtype=f)
    cbf[:, 1] = 1.0 / 1024.0
    cbf[0:64, 2, 0:64] = 1.0 / 64.0
    cbf[64:128, 2, 64:128] = 1.0 / 64.0
    cf = np.zeros((P, 3, P), f)
    cf[:, 0] = np.eye(P, dtype=f)
    cf[:, 1] = 1.0
    cf[:, 2] = np.triu(np.ones((P, P), f), 1)
    io = np.zeros((P, cfg.CAP + 4), f)
    io[:, 0:cfg.CAP] = np.arange(cfg.CAP, dtype=f)[None, :]
    for b_ in range(4):
        io[:, cfg.CAP + b_] = np.arange(P, dtype=f) + 128.0 * b_
    sj = np.arange(P)[:, None].astype(np.float64)
    qi = np.arange(P)[None, :].astype(np.float64)
    eb = np.zeros((P, 8, 256), np.float64)
    for h in range(8):
        slope = 2.0 ** (-(h + 1))
        dprev = 128.0 + qi - sj
        eb[:, h, 0:128] = np.where(dprev < 128.0, np.exp(-slope * dprev), 0.0)
        dcur = qi - sj
        eb[:, h, 128:256] = np.where(dcur >= 0.0, np.exp(-slope * dcur), 0.0)
    ebd = np.zeros((P, 4, 512), np.float64)
    for h in range(8):
        g_, hh_ = h // 4, h % 4
        ebd[:, 2 * g_ + hh_ % 2, (hh_ // 2) * 256:(hh_ // 2 + 1) * 256] = eb[:, h, :]
    sh = {"prm": prm, "cbf": cbf.reshape(P, -1), "cf": cf.reshape(P, -1), "eb": ebd.astype(f).reshape(P, -1), "io": io}

    w_in = np.asarray(inp["hy_w_in"], f)
    q, k, v, rest = w_in[..., 0:512], w_in[..., 512:640], w_in[..., 640:768], w_in[..., 768:]
    k0, k1 = k[..., 0:64], k[..., 64:128]
    sh["hy_w_in"] = np.ascontiguousarray(np.concatenate([q, k0, k0, k1, k1, v, rest], axis=-1))
    sh["hy_w_out"] = np.ascontiguousarray(inp["hy_w_out"], f)
    sh["ffn_w_gate"] = np.ascontiguousarray(inp["ffn_w_gate"], f)
    sh["ffn_w_up"] = np.ascontiguousarray(inp["ffn_w_up"], f)
    sh["ffn_w_down"] = np.ascontiguousarray(inp["ffn_w_down"], f)
    if cfg.n_odd:
        rw = np.asarray(inp["rg_w_in"], f)
        y, xx = rw[..., 0:1024].reshape(-1, D, 8, 128), rw[..., 1024:].reshape(-1, D, 8, 128)
        sh["rg_w_in"] = np.ascontiguousarray(np.stack([y, xx], axis=3).reshape(-1, D, 2048))
        sh["rg_ga"] = np.ascontiguousarray(inp["rg_gate_a_w"], f)
        sh["rg_gx"] = np.ascontiguousarray(inp["rg_gate_x_w"], f)
        sh["rg_w_out"] = np.ascontiguousarray(inp["rg_w_out"], f)
        r = np.asarray(inp["moe_router"], f).reshape(cfg.n_odd, KC, P, cfg.NE)
        sh["rtr"] = np.ascontiguousarray(np.transpose(r, (2, 0, 1, 3)).reshape(P, -1))
        sh["moe_w_gate"] = np.ascontiguousarray(inp["moe_w_gate"], f)
        sh["moe_w_up"] = np.ascontiguousarray(inp["moe_w_up"], f)
        sh["moe_w_down"] = np.ascontiguousarray(inp["moe_w_down"], f)
    for key in ("hy_w_in", "hy_w_out", "ffn_w_gate", "ffn_w_up", "ffn_w_down", "rg_w_in", "rg_ga", "rg_gx", "rg_w_out",
                "moe_w_gate", "moe_w_up", "moe_w_down"):
        if key in sh:
            a = sh.pop(key)
            for jj in range(a.shape[0]):
                sh["%s_%d" % (key, jj)] = np.ascontiguousarray(a[jj])
    return sh


class StopEmit(Exception):
    pass


class Buf:
    __slots__ = ("w", "r")

    def __init__(self):
        self.w = None
        self.r = {}


class Ctx:
    def __init__(self, nc, dry):
        self.nc = nc
        self.dry = dry
        self.eng = {"pe": nc.tensor, "act": nc.scalar, "dve": nc.vector, "pool": nc.gpsimd, "sp": nc.sync}
        self.sem = {}
        self.cnt = {}
        self.known = {e: {} for e in self.eng}
        for e in self.eng:
            self.new_sem(e)
        self.pi = 0
        self.reserved = set()
        self.nops = 0
        self.skip = False

    def new_sem(self, key):
        self.sem[key] = None if self.dry else self.nc.alloc_semaphore(name="sem_%s" % (key if isinstance(key, str) else "_".join(map(str, key))))
        self.cnt[key] = 0
        return key

    def _wait(self, e, deps):
        kn = self.known[e]
        for key, val in deps.items():
            if val <= 0 or kn.get(key, 0) >= val:
                continue
            self.eng[e].wait_ge(self.sem[key], val)
            kn[key] = val

    def _deps(self, e, reads, writes):
        deps = {}
        for b in reads:
            if b.w is not None and deps.get(b.w[0], 0) < b.w[1]:
                deps[b.w[0]] = b.w[1]
        for b in writes:
            if b.w is not None and deps.get(b.w[0], 0) < b.w[1]:
                deps[b.w[0]] = b.w[1]
            for k, v in b.r.items():
                if k != e and deps.get(k, 0) < v:
                    deps[k] = v
        if e == "pe":
            deps.pop("pe", None)
        return deps

    def op(self, e, fn, reads=(), writes=(), inc=True):
        if self.dry or self.skip:
            return
        self.nops += 1
        self._wait(e, self._deps(e, reads, writes))
        ins = fn(self.eng[e])
        if inc:
            self.cnt[e] += 1
            ins.then_inc(self.sem[e], 1)
            val = self.cnt[e]
        else:
            val = self.cnt[e] + 1
        for b in writes:
            b.w = (e, val)
            b.r = {}
        for b in reads:
            if b.r.get(e, 0) < val:
                b.r[e] = val

    def dma(self, q, out, in_, sem_key, reads=(), writes=()):
        if self.dry or self.skip:
            return
        self.nops += 1
        self._wait(q, self._deps("dma", reads, writes))
        ins = self.eng[q].dma_start(out=out, in_=in_)
        self.cnt[sem_key] += 16
        ins.then_inc(self.sem[sem_key], 16)
        val = self.cnt[sem_key]
        for b in writes:
            b.w = (sem_key, val)
            b.r = {}
        for b in reads:
            if b.r.get(sem_key, 0) < val:
                b.r[sem_key] = val

    def barrier(self):
        if self.dry or self.skip:
            return
        for e in self.eng:
            self._wait(e, dict(self.cnt))

    def ps_next(self, n=1):
        while True:
            if n == 2 and self.pi % 2:
                self.pi += 1
            b = self.pi % 8
            if any(((b + i) % 8) in self.reserved for i in range(n)):
                self.pi += 1
                continue
            self.pi += n
            return b


class WStream:
    def __init__(self, ctx, dr, ring, nslots, specs):
        self.ctx, self.dr, self.ring, self.n = ctx, dr, ring, nslots
        self.specs = specs
        self.rec = []
        self.i = 0
        self.issued = 0
        self.bufs = [Buf() for _ in range(nslots)]
        self.keys = [ctx.new_sem(("w", s)) for s in range(nslots)]

    def _views(self, spec, s):
        kind, name, pre = spec[0], spec[1], spec[2]
        t = self.dr["%s_%d" % (name, pre[0])]
        a = t[pre[1:]] if len(pre) > 1 else t
        slot = self.ring[:, s, :]
        if kind == "cols":
            c0, ncol = spec[3], spec[4]
            return a[:, c0:c0 + ncol].rearrange("(k p) c -> p k c", p=P), slot[:, 0:KC * ncol].rearrange("p (k c) -> p k c", k=KC)
        r0, nf = spec[3], spec[4]
        return a[r0:r0 + nf * P, :].rearrange("(f p) c -> p f c", p=P), slot[:, 0:nf * D].rearrange("p (f c) -> p f c", f=nf)

    def get(self, spec):
        if self.specs is None:
            self.rec.append(spec)
            s = (len(self.rec) - 1) % self.n
            return self._views(spec, s)[1], self.bufs[s]
        i = self.i
        self.i += 1
        assert self.specs[i] == spec, (i, self.specs[i], spec)
        if self.ctx.skip:
            return self._views(spec, i % self.n)[1], self.bufs[i % self.n]
        while self.issued < min(len(self.specs), i + self.n - 3):
            jj = self.issued
            s = jj % self.n
            src, dst = self._views(self.specs[jj], s)
            self.ctx.dma("pool", out=dst, in_=src, sem_key=self.keys[s], writes=[self.bufs[s]])
            self.issued += 1
        s = i % self.n
        return self._views(spec, s)[1], self.bufs[s]


def emit(nc, ctx, cfg, layers, specs):
    S = cfg.S
    NT = S // 512
    NB = S // 128
    NF = cfg.DFF // 128
    FG = cfg.FG
    NE = cfg.NE
    cols, npar = param_layout(cfg)
    es = ExitStack()

    def din(name, shape):
        return nc.dram_tensor(name, list(shape), F32, kind="ExternalInput").ap()

    dr = {"x": din("x", [S, D]), "prm": din("prm", [P, npar]), "cbf": din("cbf", [P, 3 * P]),
          "cf": din("cf", [P, 3 * P]), "eb": din("eb", [P, 2048]), "io": din("io", [P, cfg.CAP + 4])}
    for jj in range(cfg.n_even):
        for nm_, shp in (("hy_w_in", [D, 2432]), ("hy_w_out", [D, D]), ("ffn_w_gate", [D, cfg.DFF]),
                         ("ffn_w_up", [D, cfg.DFF]), ("ffn_w_down", [cfg.DFF, D])):
            dr["%s_%d" % (nm_, jj)] = din("%s_%d" % (nm_, jj), shp)
    if cfg.n_odd:
        dr["rtr"] = din("rtr", [P, cfg.n_odd * KC * NE])
    for jj in range(cfg.n_odd):
        for nm_, shp in (("rg_w_in", [D, 2048]), ("rg_ga", [8, P, P]), ("rg_gx", [8, P, P]), ("rg_w_out", [D, D]),
                         ("moe_w_gate", [NE, D, cfg.DFF]), ("moe_w_up", [NE, D, cfg.DFF]), ("moe_w_down", [NE, cfg.DFF, D])):
            dr["%s_%d" % (nm_, jj)] = din("%s_%d" % (nm_, jj), shp)
    out_d = nc.dram_tensor("out", [S, D], F32, kind="ExternalOutput").ap()

    uid = [0]

    def sb(stack, name, shape, dt):
        uid[0] += 1
        return stack.enter_context(nc.sbuf_tensor("%s_%d" % (name, uid[0]), shape, dt))

    xT = sb(es, "xT", [P, KC, S], F32)
    hT = sb(es, "hT", [P, KC, S], BF16)
    xb = [Buf() for _ in range(NT)]
    hb = [Buf() for _ in range(NT)]
    ps = es.enter_context(nc.psum_tensor("ps", [P, 8, 512], F32))
    pb = [Buf() for _ in range(8)]
    ring = sb(es, "ring", [P, cfg.nslots, SLOT], BF16)
    ws = WStream(ctx, dr, ring, cfg.nslots, specs)
    sq = sb(es, "sq", [P, 2, 2, 512], BF16)
    sqb = [Buf(), Buf()]
    rstd = sb(es, "rstd", [P, 2, 512], F32)
    rsb = [Buf(), Buf()]
    cbf = sb(es, "cbf_s", [P, 3, P], BF16)
    cf = sb(es, "cf_s", [P, 3, P], F32)
    io = sb(es, "io_s", [P, cfg.CAP + 4], F32)
    eb = sb(es, "eb_s", [P, 4, 512], F32)
    prm = sb(es, "prm_s", [P, npar], F32)
    rtr = sb(es, "rtr_s", [P, max(1, cfg.n_odd) * KC * NE], F32)
    der = sb(es, "der_s", [P, 96], F32)
    cB = Buf()
    dB = Buf()
    par = [0]

    def flip():
        par[0] ^= 1
        return par[0]

    def pc(name, i):
        return prm[:, cols[name] + i:cols[name] + i + 1]

    csem = ctx.new_sem("cst")
    ctx.dma("sp", out=cf[:, :, :], in_=dr["cf"].rearrange("p (a b) -> p a b", a=3), sem_key=csem, writes=[cB])
    ctx.dma("sp", out=io[:, :], in_=dr["io"], sem_key=csem, writes=[cB])
    ctx.dma("sp", out=eb[:, :, :], in_=dr["eb"].rearrange("p (a b) -> p a b", a=4), sem_key=csem, writes=[cB])
    ctx.dma("sp", out=prm[:, :], in_=dr["prm"], sem_key=csem, writes=[cB])
    if cfg.n_odd:
        ctx.dma("sp", out=rtr[:, :], in_=dr["rtr"], sem_key=csem, writes=[cB])
    csem2 = ctx.new_sem("cst2")
    cB2 = Buf()
    ctx.dma("pool", out=cbf[:, :, :], in_=dr["cbf"].rearrange("p (a b) -> p a b", a=3), sem_key=csem2, writes=[cB2])
    ctx.op("dve", lambda e: e.memset(der[:, 80:84], EPS), reads=[cB2], writes=[cB, dB])
    ne_, no_ = cfg.n_even, cfg.n_odd
    ctx.op("dve", lambda e: e.tensor_scalar(out=der[:, 0:ne_], in0=prm[:, cols["qg"]:cols["qg"] + ne_], scalar1=0.125, scalar2=None, op0=ALU.mult),
           reads=[cB], writes=[dB])
    ctx.op("act", lambda e: e.activation(out=der[:, 8:8 + 8 * ne_], in_=prm[:, cols["snk"]:cols["snk"] + 8 * ne_], func=AF.Exp),
           reads=[cB], writes=[dB])
    if no_:
        lam = prm[:, cols["lam"]:cols["lam"] + 8 * no_]
        z = der[:, 48:48 + 8 * no_]
        c1 = der[:, 32:32 + 8 * no_]
        ctx.op("dve", lambda e: e.tensor_scalar(out=z, in0=lam, scalar1=-1.0, scalar2=None, op0=ALU.mult), reads=[cB, dB], writes=[dB])
        ctx.op("dve", lambda e: e.scalar_tensor_tensor(out=c1, in0=z, scalar=-1.0, in1=z, op0=ALU.mult, op1=ALU.max), reads=[dB], writes=[dB])
        ctx.op("act", lambda e: e.activation(out=c1, in_=c1, func=AF.Exp, scale=-1.0), reads=[dB], writes=[dB])
        ctx.op("act", lambda e: e.activation(out=c1, in_=c1, func=AF.Ln, bias=1.0), reads=[dB], writes=[dB])
        ctx.op("dve", lambda e: e.scalar_tensor_tensor(out=c1, in0=z, scalar=0.0, in1=c1, op0=ALU.max, op1=ALU.add), reads=[dB], writes=[dB])
        ctx.op("dve", lambda e: e.tensor_scalar(out=c1, in0=c1, scalar1=-8.0, scalar2=None, op0=ALU.mult), reads=[dB], writes=[dB])
        ctx.op("dve", lambda e: e.tensor_scalar(out=der[:, 64:64 + 8 * no_], in0=c1, scalar1=2.0, scalar2=None, op0=ALU.mult), reads=[dB], writes=[dB])

    def tts(tt):
        return slice(tt * 512, (tt + 1) * 512)

    def ckpt(k):
        if getattr(cfg, "stage", None) == k:
            ctx.skip = True

    def proj(wv, wbuf, c0, tt, srcs):
        b = ctx.ps_next()
        nk = len(srcs)
        for k in range(nk):
            t_, bk, kk = srcs[k]
            ctx.op("pe", lambda e: e.matmul(ps[:, b, :], lhsT=wv[:, k, c0:c0 + P], rhs=t_[:, kk, tts(tt)], start=(k == 0), stop=(k == nk - 1)),
                   reads=[wbuf, bk[tt]], writes=[pb[b]], inc=(k == nk - 1))
        return b

    hsrc = [(hT, hb, k) for k in range(KC)]
    htok = hT[:, :, :].rearrange("p c s -> p (c s)").rearrange("p (i f) -> p i f", f=D)

    with ExitStack() as ph:
        xin = [sb(ph, "xin%d" % i, [P, D], F32) for i in range(2)]
        xinb = [Buf(), Buf()]
        xsem = [ctx.new_sem(("xin", i)) for i in range(2)]
        for i in range(NB):
            k = i % 2
            ctx.dma("sp", out=xin[k][:, :], in_=dr["x"][i * P:(i + 1) * P, :], sem_key=xsem[k], writes=[xinb[k]])
            for half in range(2):
                b = ctx.ps_next()
                for c4 in range(4):
                    c = half * 4 + c4
                    ctx.op("pe", lambda e: e.transpose(out=ps[:, b, c4 * P:(c4 + 1) * P], in_=xin[k][:, c * P:(c + 1) * P], identity=cf[:, 0, :]),
                           reads=[xinb[k], cB], writes=[pb[b]], inc=(c4 == 3))
                src = ps[:, b, :].rearrange("p (c t) -> p c t", c=4)
                dst = xT[:, half * 4:(half + 1) * 4, i * P:(i + 1) * P]
                if half == 0:
                    ctx.op("act", lambda e: e.copy(out=dst, in_=src), reads=[pb[b]], writes=[xb[i // 4]])
                else:
                    ctx.op("dve", lambda e: e.tensor_copy(out=dst, in_=src), reads=[pb[b]], writes=[xb[i // 4]])
        ctx.barrier()

    def rmsnorm(gname, l, router=None):
        for tt in range(NT):
            k = flip()
            b = ctx.ps_next()
            for h2 in range(4):
                kq = flip()
                ctx.op("act", lambda e: e.activation(out=sq[:, kq, :, :], in_=xT[:, h2 * 2:(h2 + 1) * 2, tts(tt)], func=AF.Square),
                       reads=[xb[tt]], writes=[sqb[kq]])
                for c4 in range(2):
                    c = h2 * 2 + c4
                    ctx.op("pe", lambda e: e.matmul(ps[:, b, :], lhsT=cbf[:, 1, :], rhs=sq[:, kq, c4, :], start=(c == 0), stop=(c == 7)),
                           reads=[sqb[kq], cB], writes=[pb[b]], inc=(c4 == 1))
            ctx.op("act", lambda e: e.activation(out=rstd[:, k, :], in_=ps[:, b, :], func=AF.Sqrt, bias=der[:, 80:81]), reads=[pb[b], dB], writes=[rsb[k]])
            ctx.op("dve", lambda e: e.reciprocal(out=rstd[:, k, :], in_=rstd[:, k, :]), reads=[rsb[k]], writes=[rsb[k]])
            for c in range(KC if (router is None or not cfg.sparse) else 0):
                ctx.op("dve", lambda e: e.scalar_tensor_tensor(out=hT[:, c, tts(tt)], in0=xT[:, c, tts(tt)], scalar=pc(gname, l * 8 + c),
                                                               in1=rstd[:, k, :], op0=ALU.mult, op1=ALU.mult),
                       reads=[xb[tt], rsb[k], cB], writes=[hb[tt]])
            if router is not None:
                j, hf, hfb, lgb = router
                for i4 in range(4):
                    i = tt * 4 + i4
                    tsl = slice(i * P, (i + 1) * P)
                    for c in range(KC):
                        ctx.op("dve", lambda e: e.scalar_tensor_tensor(out=hf[:, c, :], in0=xT[:, c, tsl], scalar=pc(gname, l * 8 + c),
                                                                       in1=rstd[:, k, i4 * P:(i4 + 1) * P], op0=ALU.mult, op1=ALU.mult),
                               reads=[xb[tt], rsb[k], cB], writes=[hfb])
                    for c in range(KC):
                        o = (j * KC + c) * NE
                        ctx.op("pe", lambda e: e.matmul(ps[:, lgb, i * NE:(i + 1) * NE], lhsT=hf[:, c, :], rhs=rtr[:, o:o + NE], start=(c == 0), stop=(c == 7)),
                               reads=[hfb, cB], writes=[pb[lgb]], inc=(c == 7))
                    if cfg.sparse:
                        for half in range(2):
                            bT = ctx.ps_next()
                            for c4 in range(4):
                                ctx.op("pe", lambda e: e.transpose(out=ps[:, bT, c4 * P:(c4 + 1) * P], in_=hf[:, half * 4 + c4, :], identity=cf[:, 0, :]),
                                       reads=[hfb, cB], writes=[pb[bT]], inc=(c4 == 3))
                            ctx.op("act", lambda e: e.copy(out=htok[:, i, half * 512:(half + 1) * 512], in_=ps[:, bT, :]), reads=[pb[bT]], writes=[hb[tt]])

    def out_proj(wname, j, srcs):
        for op_ in range(4):
            wv, wbuf = ws.get(("cols", wname, (j,), op_ * 256, 256))
            for o2 in range(2):
                oc = op_ * 2 + o2
                for tt in range(NT):
                    b = proj(wv, wbuf, o2 * P, tt, srcs)
                    ctx.op("dve", lambda e: e.tensor_tensor(out=xT[:, oc, tts(tt)], in0=ps[:, b, :], in1=xT[:, oc, tts(tt)], op=ALU.add),
                           reads=[pb[b], xb[tt]], writes=[xb[tt]])

    def even_mixer(j, l):
        with ExitStack() as ph:
            qT = sb(ph, "qT", [P, 4, S], BF16)
            kd = sb(ph, "kd", [P, 2, S], BF16)
            va = sb(ph, "va", [P, NB, 2, 65], BF16)
            cvT = sb(ph, "cvT", [P, 4, S], BF16)
            cu = sb(ph, "cu", [P, 2, 514], F32)
            acc = sb(ph, "acc", [P, 2, 512], F32)
            E = sb(ph, "E", [P, 2, 512], F32)
            eT = sb(ph, "eT", [P, 2, 2, 512], BF16)
            den = sb(ph, "den", [P, 2, 8], F32)
            atok = sb(ph, "atok", [P, 2, 4, 64], F32)
            qb = [Buf() for _ in range(NT)]
            kb = [Buf() for _ in range(NT)]
            vb = [Buf() for _ in range(NT)]
            cvb = [Buf() for _ in range(NT)]
            cub = [Buf(), Buf()]
            accb = [Buf(), Buf()]
            Eb = Buf()
            eTb = [Buf(), Buf()]
            denb = Buf()
            atb = Buf()
            vone = Buf()
            ctx.op("dve", lambda e: e.memset(va[:, :, :, 64:65], 1.0), writes=vb)

            rmsnorm("nm", l)
            ckpt(2)

            def qknorm(b, dst, dstbuf, gcol):
                k = flip()
                ctx.op("act", lambda e: e.activation(out=sq[:, k, 0, :], in_=ps[:, b, :], func=AF.Square), reads=[pb[b]], writes=[sqb[k]])
                b2 = ctx.ps_next()
                ctx.op("pe", lambda e: e.matmul(ps[:, b2, :], lhsT=cbf[:, 2, :], rhs=sq[:, k, 0, :], start=True, stop=True),
                       reads=[sqb[k], cB], writes=[pb[b2]])
                ctx.op("act", lambda e: e.activation(out=rstd[:, k, :], in_=ps[:, b2, :], func=AF.Sqrt, bias=der[:, 80:81]), reads=[pb[b2], dB], writes=[rsb[k]])
                ctx.op("dve", lambda e: e.reciprocal(out=rstd[:, k, :], in_=rstd[:, k, :]), reads=[rsb[k]], writes=[rsb[k]])
                ctx.op("dve", lambda e: e.scalar_tensor_tensor(out=dst, in0=ps[:, b, :], scalar=gcol, in1=rstd[:, k, :], op0=ALU.mult, op1=ALU.mult),
                       reads=[pb[b], rsb[k], cB, dB], writes=[dstbuf])

            for ld in range(3):
                wv, wbuf = ws.get(("cols", "hy_w_in", (j,), ld * 256, 256))
                for o2 in range(2):
                    c = ld * 2 + o2
                    for tt in range(NT):
                        b = proj(wv, wbuf, o2 * P, tt, hsrc)
                        if c < 4:
                            qknorm(b, qT[:, c, tts(tt)], qb[tt], der[:, j:j + 1])
                        else:
                            qknorm(b, kd[:, c - 4, tts(tt)], kb[tt], pc("kg", j))
            ckpt(3)
            wv, wbuf = ws.get(("cols", "hy_w_in", (j,), 768, 128))
            for i in range(NB):
                b = ctx.ps_next()
                for k in range(KC):
                    ctx.op("pe", lambda e: e.matmul(ps[:, b, 0:P], lhsT=hT[:, k, i * P:(i + 1) * P], rhs=wv[:, k, :], start=(k == 0), stop=(k == 7)),
                           reads=[wbuf, hb[i // 4]], writes=[pb[b]], inc=(k == 7))
                ctx.op("act", lambda e: e.copy(out=va[:, i, :, 0:64], in_=ps[:, b, 0:P].rearrange("p (g d) -> p g d", g=2)),
                       reads=[pb[b]], writes=[vb[i // 4]])
            ckpt(4)
            for pcp in range(2):
                wgc, bgc = ws.get(("cols", "hy_w_in", (j,), 1408 + pcp * 256, 256))
                wu, bu_ = ws.get(("cols", "hy_w_in", (j,), 1920 + pcp * 256, 256))
                wgb, bgb = ws.get(("cols", "hy_w_in", (j,), 896 + pcp * 256, 256))
                for c2 in range(2):
                    c = pcp * 2 + c2
                    for tt in range(NT):
                        k = flip()
                        bc = proj(wgc, bgc, c2 * P, tt, hsrc)
                        bu = proj(wu, bu_, c2 * P, tt, hsrc)
                        if tt == 0:
                            ctx.op("dve", lambda e: e.memset(cu[:, k, 0:2], 0.0), writes=[cub[k]])
                        else:
                            ctx.op("dve", lambda e: e.tensor_copy(out=cu[:, k, 0:2], in_=cu[:, 1 - k, 512:514]), reads=[cub[1 - k]], writes=[cub[k]])
                        ctx.op("act", lambda e: e.copy(out=cu[:, k, 2:514], in_=ps[:, bc, :]), reads=[pb[bc]], writes=[cub[k]])
                        ctx.op("dve", lambda e: e.tensor_tensor(out=cu[:, k, 2:514], in0=ps[:, bu, :], in1=cu[:, k, 2:514], op=ALU.mult),
                               reads=[pb[bu], cub[k]], writes=[cub[k]])
                        wc = lambda kk: pc("cw", (j * 3 + kk) * 4 + c)
                        ctx.op("dve", lambda e: e.tensor_scalar(out=acc[:, k, :], in0=cu[:, k, 2:514], scalar1=wc(2), scalar2=None, op0=ALU.mult),
                               reads=[cub[k], cB], writes=[accb[k]])
                        ctx.op("dve", lambda e: e.scalar_tensor_tensor(out=acc[:, k, :], in0=cu[:, k, 1:513], scalar=wc(1), in1=acc[:, k, :], op0=ALU.mult, op1=ALU.add),
                               reads=[cub[k], accb[k], cB], writes=[accb[k]])
                        ctx.op("dve", lambda e: e.scalar_tensor_tensor(out=acc[:, k, :], in0=cu[:, k, 0:512], scalar=wc(0), in1=acc[:, k, :], op0=ALU.mult, op1=ALU.add),
                               reads=[cub[k], accb[k], cB], writes=[accb[k]])
                        bb = proj(wgb, bgb, c2 * P, tt, hsrc)
                        ctx.op("dve", lambda e: e.tensor_tensor(out=cvT[:, c, tts(tt)], in0=ps[:, bb, :], in1=acc[:, k, :], op=ALU.mult),
                               reads=[pb[bb], accb[k]], writes=[cvb[tt]])
            ckpt(5)
            esk = der[:, 8 + 8 * j:16 + 8 * j]
            for n in range(NB):
                bO = ctx.ps_next(2)
                qs = slice(n * P, (n + 1) * P)
                for g in range(2):
                    bS = ctx.ps_next(2)
                    k = flip()
                    for hh in range(4):
                        h = 4 * g + hh
                        hf_ = 0 if getattr(cfg, 'nohalf', False) else h % 2
                        rq = qT[hf_ * 64:(hf_ + 1) * 64, h // 2, qs]
                        so = (hh // 2) * 256
                        if n > 0:
                            ctx.op("pe", lambda e: e.matmul(ps[:, bS + hh % 2, so:so + P], lhsT=kd[hf_ * 64:(hf_ + 1) * 64, g, (n - 1) * P:n * P], rhs=rq, start=True, stop=True),
                                   reads=[kb[(n - 1) // 4], qb[n // 4]], writes=[pb[bS + hh % 2]], inc=False)
                        ctx.op("pe", lambda e: e.matmul(ps[:, bS + hh % 2, so + P:so + 2 * P], lhsT=kd[hf_ * 64:(hf_ + 1) * 64, g, qs], rhs=rq, start=True, stop=True),
                               reads=[kb[n // 4], qb[n // 4]], writes=[pb[bS + hh % 2]], inc=(hh == 3))
                    for b2_ in range(2):
                        ctx.op("act", lambda e: e.activation(out=E[:, b2_, :], in_=ps[:, bS + b2_, :], func=AF.Exp), reads=[pb[bS + b2_]], writes=[Eb])
                    ctx.op("dve", lambda e: e.tensor_tensor(out=eT[:, k, :, :], in0=E[:, :, :], in1=eb[:, 2 * g:2 * g + 2, :], op=ALU.mult),
                           reads=[Eb, cB], writes=[eTb[k]])
                    ckpt(51)
                    for hh in range(4):
                        h = 4 * g + hh
                        so = (hh // 2) * 256
                        oh = ps[:, bO + h // 4, (h % 4) * P:(h % 4) * P + 65]
                        if n > 0:
                            ctx.op("pe", lambda e: e.matmul(oh, lhsT=eT[:, k, hh % 2, so:so + P], rhs=va[:, n - 1, g, :], start=True, stop=False),
                                   reads=[eTb[k], vb[(n - 1) // 4]], writes=[pb[bO + h // 4]], inc=False)
                        ctx.op("pe", lambda e: e.matmul(oh, lhsT=eT[:, k, hh % 2, so + P:so + 2 * P], rhs=va[:, n, g, :], start=(n == 0), stop=True),
                               reads=[eTb[k], vb[n // 4]], writes=[pb[bO + h // 4]], inc=(hh == 3))
                ckpt(52)
                Ov = ps[:, bO:bO + 2, :].rearrange("p b (h d) -> p b h d", h=4)
                dn = den[:, 0, :].rearrange("p (b h) -> p b h", b=2)
                rc = den[:, 1, :].rearrange("p (b h) -> p b h", b=2)
                ctx.op("dve", lambda e: e.tensor_tensor(out=dn, in0=Ov[:, :, :, 64], in1=esk.rearrange("p (b h) -> p b h", b=2), op=ALU.add),
                       reads=[pb[bO], pb[bO + 1], dB], writes=[denb])
                ctx.op("dve", lambda e: e.reciprocal(out=den[:, 1, :], in_=den[:, 0, :]), reads=[denb], writes=[denb])
                ctx.op("dve", lambda e: e.tensor_tensor(out=atok[:, :, :, :], in0=Ov[:, :, :, 0:64], in1=rc.unsqueeze(3).broadcast_to([P, 2, 4, 64]), op=ALU.mult),
                       reads=[pb[bO], pb[bO + 1], denb], writes=[atb])
                ckpt(53)
                bT = ctx.ps_next()
                a2 = atok[:, :, :, :].rearrange("p b h d -> p (b h d)")
                for c in range(4):
                    ctx.op("pe", lambda e: e.transpose(out=ps[:, bT, c * P:(c + 1) * P], in_=a2[:, c * P:(c + 1) * P], identity=cf[:, 0, :]),
                           reads=[atb, cB], writes=[pb[bT]], inc=(c == 3))
                ctx.op("act", lambda e: e.copy(out=hT[:, 0:4, qs], in_=ps[:, bT, :].rearrange("p (c t) -> p c t", c=4)),
                       reads=[pb[bT]], writes=[hb[n // 4]])
            ckpt(6)
            out_proj("hy_w_out", j, [(hT, hb, k) for k in range(4)] + [(cvT, cvb, k) for k in range(4)])
            ctx.barrier()

    def odd_mixer(j, l):
        with ExitStack() as ph:
            mixT = sb(ph, "mixT", [P, KC, S], BF16)
            gw = sb(ph, "gw", [P, 2, 8, P], BF16)
            xbuf = sb(ph, "xbuf", [P, 2, 515], F32)
            tmp = sb(ph, "tmp", [P, 9, 512], F32)
            xcb = sb(ph, "xcb", [P, 512], BF16)
            hs = sb(ph, "hs", [P, 2, 512], F32)
            mb = [Buf() for _ in range(NT)]
            gwb = Buf()
            xbb = [Buf(), Buf()]
            tb = [Buf() for _ in range(9)]
            xcbb = Buf()
            hsb = [Buf(), Buf()]
            gsem = ctx.new_sem(("gw", l))
            ctx.dma("pool", out=gw[:, 0, :, :], in_=dr["rg_ga_%d" % j].rearrange("h i j -> i h j"), sem_key=gsem, writes=[gwb])
            ctx.dma("pool", out=gw[:, 1, :, :], in_=dr["rg_gx_%d" % j].rearrange("h i j -> i h j"), sem_key=gsem, writes=[gwb])
            rmsnorm("nm", l)
            T1, SG, YG, XC, R, IG, A, TT_, BB = range(9)
            HW_ = 256
            tbs = [[Buf() for _ in range(9)] for _ in range(2)]
            xcbs = [Buf(), Buf()]
            hss = [[Buf(), Buf()], [Buf(), Buf()]]
            for c in range(KC):
                wv, wbuf = ws.get(("cols", "rg_w_in", (j,), c * 256, 256))
                for tt in range(NT):
                    k = flip()
                    by = proj(wv, wbuf, 0, tt, hsrc)
                    bx = proj(wv, wbuf, P, tt, hsrc)
                    if tt == 0:
                        ctx.op("dve", lambda e: e.memset(xbuf[:, k, 0:3], 0.0), writes=[xbb[k]])
                    else:
                        ctx.op("dve", lambda e: e.tensor_copy(out=xbuf[:, k, 0:3], in_=xbuf[:, 1 - k, 512:515]), reads=[xbb[1 - k]], writes=[xbb[k]])
                    ctx.op("act", lambda e: e.copy(out=xbuf[:, k, 3:515], in_=ps[:, bx, :]), reads=[pb[bx]], writes=[xbb[k]])
                    wc = lambda kk: pc("rcw", (j * 4 + kk) * 8 + c)
                    gbank = [[ctx.ps_next(), ctx.ps_next()] for _ in range(2)]
                    steps = []

                    def chain(sg_):
                        cs = slice(sg_ * HW_, (sg_ + 1) * HW_)
                        py = ps[:, by, cs]
                        tb_ = tbs[sg_]
                        t = lambda i: tmp[:, i, cs]
                        xs = lambda kk: xbuf[:, k, kk + sg_ * HW_:kk + (sg_ + 1) * HW_]
                        br, bi = gbank[sg_]
                        yield ("act", lambda e: e.activation(out=t(T1), in_=py, func=AF.Square), [pb[by]], [tb_[T1]])
                        yield ("dve", lambda e: e.tensor_scalar(out=t(T1), in0=t(T1), scalar1=0.044715, scalar2=1.0, op0=ALU.mult, op1=ALU.add), [tb_[T1]], [tb_[T1]])
                        yield ("dve", lambda e: e.tensor_tensor(out=t(T1), in0=py, in1=t(T1), op=ALU.mult), [pb[by], tb_[T1]], [tb_[T1]])
                        yield ("dve", lambda e: e.tensor_scalar(out=t(XC), in0=xs(3), scalar1=wc(3), scalar2=pc("rcb", j * 8 + c), op0=ALU.mult, op1=ALU.add),
                               [xbb[k], cB], [tb_[XC]])
                        yield ("act", lambda e: e.activation(out=t(SG), in_=t(T1), func=AF.Sigmoid, scale=1.5957691216057308), [tb_[T1]], [tb_[SG]])
                        for kk in (2, 1, 0):
                            yield ("dve", (lambda kk: (lambda e: e.scalar_tensor_tensor(out=t(XC), in0=xs(kk), scalar=wc(kk), in1=t(XC), op0=ALU.mult, op1=ALU.add)))(kk),
                                   [xbb[k], tb_[XC], cB], [tb_[XC]])
                        yield ("act", lambda e: e.copy(out=xcb[:, cs], in_=t(XC)), [tb_[XC]], [xcbs[sg_]])
                        yield ("dve", lambda e: e.tensor_tensor(out=t(YG), in0=py, in1=t(SG), op=ALU.mult), [pb[by], tb_[SG]], [tb_[YG]])
                        yield ("pe", lambda e: e.matmul(ps[:, br, 0:HW_], lhsT=gw[:, 0, c, :], rhs=xcb[:, cs], start=True, stop=True), [gwb, xcbs[sg_]], [pb[br]])
                        yield ("pe", lambda e: e.matmul(ps[:, bi, 0:HW_], lhsT=gw[:, 1, c, :], rhs=xcb[:, cs], start=True, stop=True), [gwb, xcbs[sg_]], [pb[bi]])
                        yield ("act", lambda e: e.activation(out=t(R), in_=ps[:, br, 0:HW_], func=AF.Sigmoid, bias=pc("gab", j * 8 + c)), [pb[br], cB], [tb_[R]])
                        yield ("act", lambda e: e.activation(out=t(IG), in_=ps[:, bi, 0:HW_], func=AF.Sigmoid, bias=pc("gxb", j * 8 + c)), [pb[bi], cB], [tb_[IG]])
                        yield ("act", lambda e: e.activation(out=t(A), in_=t(R), func=AF.Exp, scale=der[:, 32 + j * 8 + c:33 + j * 8 + c]), [tb_[R], dB], [tb_[A]])
                        yield ("act", lambda e: e.activation(out=t(TT_), in_=t(R), func=AF.Exp, scale=der[:, 64 + j * 8 + c:65 + j * 8 + c]), [tb_[R], dB], [tb_[TT_]])
                        yield ("dve", lambda e: e.tensor_tensor(out=t(BB), in0=t(IG), in1=t(XC), op=ALU.mult), [tb_[IG], tb_[XC]], [tb_[BB]])
                        yield ("act", lambda e: e.activation(out=t(TT_), in_=t(TT_), func=AF.Relu, scale=-1.0, bias=1.0), [tb_[TT_]], [tb_[TT_]])
                        yield ("act", lambda e: e.activation(out=t(TT_), in_=t(TT_), func=AF.Sqrt), [tb_[TT_]], [tb_[TT_]])
                        if tt == 0 and sg_ == 0:
                            yield ("dve", lambda e: e.memset(tmp[:, TT_, 0:1], 1.0), [tb_[TT_]], [tb_[TT_]])
                        elif tt == 0:
                            yield None
                        yield ("dve", lambda e: e.tensor_tensor(out=t(BB), in0=t(BB), in1=t(TT_), op=ALU.mult), [tb_[BB], tb_[TT_]], [tb_[BB]])
                        if sg_ == 0:
                            init = 0.0 if tt == 0 else hs[:, 1 - k, 511:512]
                            prevb = hss[1 - k][1]
                        else:
                            init = hs[:, k, HW_ - 1:HW_]
                            prevb = hss[k][0]
                        yield ("dve", lambda e: e.tensor_tensor_scan(out=hs[:, k, cs], data0=t(A), data1=t(BB), initial=init, op0=ALU.mult, op1=ALU.add),
                               [tb_[A], tb_[BB], prevb], [hss[k][sg_]])
                        yield ("dve", lambda e: e.tensor_tensor(out=mixT[:, c, tt * 512 + sg_ * HW_:tt * 512 + (sg_ + 1) * HW_], in0=t(YG), in1=hs[:, k, cs], op=ALU.mult),
                               [tb_[YG], hss[k][sg_]], [mb[tt]])

                    g0_, g1_ = chain(0), chain(1)
                    done = [False, False]
                    while not all(done):
                        for gi, gen in enumerate((g0_, g1_)):
                            if done[gi]:
                                continue
                            try:
                                item = next(gen)
                                if item is not None:
                                    ctx.op(item[0], item[1], reads=item[2], writes=item[3])
                            except StopIteration:
                                done[gi] = True
            out_proj("rg_w_out", j, [(mixT, mb, k) for k in range(KC)])
            ctx.barrier()

    actpar = [0]

    dpend = [None]

    def dense_down(ka, wd, bwd, actb, actB):
        for oc in range(KC):
            for tt in range(NT):
                b = ctx.ps_next()
                for f in range(FG):
                    ctx.op("pe", lambda e: e.matmul(ps[:, b, :], lhsT=wd[:, f, oc * P:(oc + 1) * P], rhs=actb[:, ka, f, tts(tt)], start=(f == 0), stop=(f == FG - 1)),
                           reads=[bwd, actB[ka][tt]], writes=[pb[b]], inc=(f == FG - 1))
                ctx.op("dve", lambda e: e.tensor_tensor(out=xT[:, oc, tts(tt)], in0=ps[:, b, :], in1=xT[:, oc, tts(tt)], op=ALU.add),
                       reads=[pb[b], xb[tt]], writes=[xb[tt]])

    def swiglu(names, pre, loc, G=None, Gb=None):
        actb, actB, sg, sgb, tm, tmb = loc
        for g in range(NF // FG):
            wg, bwg = ws.get(("cols", names[0], pre, g * FG * P, FG * P))
            wu, bwu = ws.get(("cols", names[1], pre, g * FG * P, FG * P))
            wd, bwd = ws.get(("rows", names[2], pre, g * FG * P, FG))
            actpar[0] ^= 1
            ka = actpar[0]
            for f in range(FG):
                for tt in range(NT):
                    k = flip()
                    bg = proj(wg, bwg, f * P, tt, hsrc)
                    bu = proj(wu, bwu, f * P, tt, hsrc)
                    ctx.op("act", lambda e: e.activation(out=sg[:, k, :], in_=ps[:, bg, :], func=AF.Silu), reads=[pb[bg]], writes=[sgb[k]])
                    if G is None:
                        ctx.op("dve", lambda e: e.tensor_tensor(out=actb[:, ka, f, tts(tt)], in0=ps[:, bu, :], in1=sg[:, k, :], op=ALU.mult),
                               reads=[pb[bu], sgb[k]], writes=[actB[ka][tt]])
                    else:
                        ctx.op("dve", lambda e: e.tensor_tensor(out=tm[:, k, :], in0=ps[:, bu, :], in1=sg[:, k, :], op=ALU.mult),
                               reads=[pb[bu], sgb[k]], writes=[tmb[k]])
                        ctx.op("dve", lambda e: e.tensor_tensor(out=actb[:, ka, f, tts(tt)], in0=tm[:, k, :], in1=G[:, tts(tt)], op=ALU.mult),
                               reads=[tmb[k], Gb], writes=[actB[ka][tt]])
            if dpend[0] is not None:
                dense_down(*dpend[0])
            dpend[0] = (ka, wd, bwd, actb, actB)
        dense_down(*dpend[0])
        dpend[0] = None

    def ffn_locals(ph):
        actb = sb(ph, "actb", [P, 2, FG, S], BF16)
        actB = [[Buf() for _ in range(NT)] for _ in range(2)]
        sg = sb(ph, "sg", [P, 2, 512], F32)
        tm = sb(ph, "tm", [P, 2, 512], F32)
        return actb, actB, sg, [Buf(), Buf()], tm, [Buf(), Buf()]

    def dense_ffn(j, l):
        with ExitStack() as ph:
            loc = ffn_locals(ph)
            rmsnorm("nf", l)
            swiglu(("ffn_w_gate", "ffn_w_up", "ffn_w_down"), (j,), loc)
            ctx.barrier()

    def moe_sparse(j, l):
        CAP = cfg.CAP
        NBK = CAP // P
        HB = NB // 2
        HS = S // 2
        with ExitStack() as ph:
            hf = sb(ph, "hf", [P, KC, P], F32)
            hfb = Buf()
            rt = sb(ph, "rt", [P, 8, NB, NE], F32)
            rs_ = sb(ph, "rs_", [P, 6, NB], F32)
            cno = sb(ph, "cno", [P, 2, NB, NE], F32)
            rb = Buf()
            Dg = hf[:, 0:HB, :]
            Dgb = hfb
            rows = sb(ph, "rows", [P, 2, HS], F32)
            rowb = [Buf(), Buf()]
            hg = sb(ph, "hg", [P, 2, KC, CAP], BF16)
            hgb = [Buf(), Buf()]
            yacc = sb(ph, "yacc", [P, 2, NBK, D], F32)
            yaccB = [[Buf() for _ in range(NBK)] for _ in range(2)]
            ybf = hg[:, 0, :, :].rearrange("p k c -> p (k c)").rearrange("p (b d) -> p b d", d=D)
            ybfB = hgb[0]
            actb = sb(ph, "actb", [P, 2, FG, CAP], BF16)
            actB = [Buf(), Buf()]
            sg = sb(ph, "sg", [P, 2, CAP], F32)
            sgb = [Buf(), Buf()]
            Sel = sb(ph, "Sel", [P, 2, CAP], BF16)
            selb = [Buf(), Buf()]
            SelT = sb(ph, "SelT", [P, 1, NBK, 512], BF16)
            stb = [Buf()]
            lgb = ctx.ps_next()
            ctx.reserved.add(lgb)
            rmsnorm("nf", l, router=(j, hf, hfb, lgb))
            L, EQ1, L2, EQ2, GT, TMP, MASK, POS = (rt[:, i, :, :] for i in range(8))
            M1, M2, DD, ED, G1, G2 = (rs_[:, i, :] for i in range(6))
            CNT, OFF = cno[:, 0, :, :], cno[:, 1, :, :]
            bc = lambda a: a.unsqueeze(2).broadcast_to([P, NB, NE])
            flat = lambda a: a.rearrange("p a b -> p (a b)")
            v3 = lambda a: a.rearrange("p (a b) -> p a b", b=NE)
            ctx.op("dve", lambda e: e.tensor_copy(out=L, in_=v3(ps[:, lgb, 0:NB * NE])), reads=[pb[lgb]], writes=[rb])
            ctx.reserved.discard(lgb)
            for fn in (
                lambda e: e.tensor_reduce(out=M1, in_=L, axis=AX.X, op=ALU.max),
                lambda e: e.tensor_tensor(out=EQ1, in0=L, in1=bc(M1), op=ALU.is_equal),
                lambda e: e.scalar_tensor_tensor(out=L2, in0=EQ1, scalar=-1e30, in1=L, op0=ALU.mult, op1=ALU.add),
                lambda e: e.tensor_reduce(out=M2, in_=L2, axis=AX.X, op=ALU.max),
                lambda e: e.tensor_tensor(out=EQ2, in0=L2, in1=bc(M2), op=ALU.is_equal),
                lambda e: e.tensor_tensor(out=DD, in0=M2, in1=M1, op=ALU.subtract),
            ):
                ctx.op("dve", fn, reads=[rb], writes=[rb])
            ctx.op("act", lambda e: e.activation(out=ED, in_=DD, func=AF.Exp), reads=[rb], writes=[rb])
            for fn in (
                lambda e: e.tensor_scalar(out=G2, in0=ED, scalar1=1.0, scalar2=None, op0=ALU.add),
                lambda e: e.reciprocal(out=G1, in_=G2),
                lambda e: e.tensor_tensor(out=G2, in0=ED, in1=G1, op=ALU.mult),
                lambda e: e.tensor_tensor(out=GT, in0=EQ1, in1=bc(G1), op=ALU.mult),
                lambda e: e.tensor_tensor(out=TMP, in0=EQ2, in1=bc(G2), op=ALU.mult),
                lambda e: e.tensor_tensor(out=GT, in0=GT, in1=TMP, op=ALU.add),
                lambda e: e.tensor_tensor(out=MASK, in0=EQ1, in1=EQ2, op=ALU.add),
            ):
                ctx.op("dve", fn, reads=[rb], writes=[rb])
            b1 = ctx.ps_next()
            ctx.op("pe", lambda e: e.matmul(ps[:, b1, 0:NB * NE], lhsT=cf[:, 2, :], rhs=flat(MASK), start=True, stop=True), reads=[rb, cB], writes=[pb[b1]])
            b2 = ctx.ps_next()
            ctx.op("pe", lambda e: e.matmul(ps[:, b2, 0:NB * NE], lhsT=cf[:, 1, :], rhs=flat(MASK), start=True, stop=True), reads=[rb, cB], writes=[pb[b2]])
            ctx.op("dve", lambda e: e.tensor_copy(out=CNT, in_=v3(ps[:, b2, 0:NB * NE])), reads=[pb[b2]], writes=[rb])
            ctx.op("dve", lambda e: e.memset(cno[:, 1, :, :], 0.0), reads=[rb], writes=[rb])
            for i in range(NB):
                if i % HB:
                    ctx.op("dve", lambda e: e.tensor_tensor(out=cno[:, 1, i, :], in0=cno[:, 1, i - 1, :], in1=cno[:, 0, i - 1, :], op=ALU.add), reads=[rb], writes=[rb])
            ctx.op("dve", lambda e: e.tensor_tensor(out=POS, in0=v3(ps[:, b1, 0:NB * NE]), in1=OFF, op=ALU.add), reads=[pb[b1], rb], writes=[rb])

            def gather(ex, hp):
                tb0 = hp * HB
                for ch in range(2):
                    banks = [ctx.ps_next() for _ in range(4)]
                    for blk in range(HB):
                        k = flip()
                        ctx.op("dve", lambda e: e.tensor_scalar(out=Sel[:, k, :], in0=io[:, 0:CAP], scalar1=rt[:, 7, tb0 + blk, ex:ex + 1],
                                                                scalar2=rt[:, 6, tb0 + blk, ex:ex + 1], op0=ALU.is_equal, op1=ALU.mult),
                               reads=[rb, cB], writes=[selb[k]])
                        for c in range(4):
                            ctx.op("pe", lambda e: e.matmul(ps[:, banks[c], 0:CAP], lhsT=htok[:, tb0 + blk, (4 * ch + c) * P:(4 * ch + c + 1) * P], rhs=Sel[:, k, :],
                                                            start=(blk == 0), stop=(blk == HB - 1)),
                                   reads=[hb[(tb0 + blk) // 4], selb[k]], writes=[pb[banks[c]]], inc=(c == 3))
                    for c in range(4):
                        ctx.op("act", lambda e: e.copy(out=hg[:, hp, 4 * ch + c, :], in_=ps[:, banks[c], 0:CAP]), reads=[pb[banks[c]]], writes=[hgb[hp]])

            def rows_build(ex, hp):
                tb0 = hp * HB
                for ri, val in ((0, GT), (1, POS)):
                    ctx.op("dve", lambda e: e.tensor_tensor(out=Dg, in0=cf[:, 0:1, :].broadcast_to([P, HB, P]),
                                                            in1=val[:, tb0:tb0 + HB, ex:ex + 1].broadcast_to([P, HB, P]), op=ALU.mult),
                           reads=[rb, cB], writes=[Dgb])
                    for q in range(HS // 512):
                        b = ctx.ps_next()
                        ctx.op("pe", lambda e: e.matmul(ps[:, b, :], lhsT=cf[:, 1, :], rhs=Dg[:, q * 4:(q + 1) * 4, :].rearrange("p a b -> p (a b)"), start=True, stop=True),
                               reads=[Dgb, cB], writes=[pb[b]])
                        ctx.op("act", lambda e: e.copy(out=rows[:, ri, q * 512:(q + 1) * 512], in_=ps[:, b, :]), reads=[pb[b]], writes=[rowb[ri]])

            def scatter(ex, hp):
                for blk in range(NBK):
                    ctx.op("act", lambda e: e.copy(out=ybf[:, blk, :], in_=yacc[:, hp, blk, :]), reads=[yaccB[hp][blk]], writes=[ybfB])
                for t2 in range(HS // 512):
                    k = 0
                    tsl = slice(t2 * 512, (t2 + 1) * 512)
                    for blk in range(NBK):
                        ctx.op("dve", lambda e: e.scalar_tensor_tensor(out=SelT[:, k, blk, :], in0=rows[:, 1, tsl], scalar=io[:, CAP + blk:CAP + blk + 1],
                                                                       in1=rows[:, 0, tsl], op0=ALU.is_equal, op1=ALU.mult),
                               reads=[rowb[0], rowb[1], cB], writes=[stb[k]])
                    tt = hp * (NT // 2) + t2
                    for c in range(KC):
                        b = ctx.ps_next()
                        for blk in range(NBK):
                            ctx.op("pe", lambda e: e.matmul(ps[:, b, :], lhsT=ybf[:, blk, c * P:(c + 1) * P], rhs=SelT[:, k, blk, :], start=(blk == 0), stop=(blk == NBK - 1)),
                                   reads=[ybfB, stb[k]], writes=[pb[b]], inc=(blk == NBK - 1))
                        ctx.op("dve", lambda e: e.tensor_tensor(out=xT[:, c, tts(tt)], in0=ps[:, b, :], in1=xT[:, c, tts(tt)], op=ALU.add),
                               reads=[pb[b], xb[tt]], writes=[xb[tt]])

            def ffn(ex):
                def emit_down(g, hp, ka, wd, bwd):
                    for blk in range(NBK):
                        for hh in range(2):
                            b = ctx.ps_next()
                            for f in range(FG):
                                ctx.op("pe", lambda e: e.matmul(ps[:, b, :], lhsT=actb[:, ka, f, blk * P:(blk + 1) * P], rhs=wd[:, f, hh * 512:(hh + 1) * 512],
                                                                start=(f == 0), stop=(f == FG - 1)),
                                       reads=[bwd, actB[ka]], writes=[pb[b]], inc=(f == FG - 1))
                            ya = yacc[:, hp, blk, hh * 512:(hh + 1) * 512]
                            if g == 0:
                                ctx.op("act", lambda e: e.copy(out=ya, in_=ps[:, b, :]), reads=[pb[b]], writes=[yaccB[hp][blk]])
                            else:
                                ctx.op("dve", lambda e: e.tensor_tensor(out=ya, in0=ps[:, b, :], in1=ya, op=ALU.add), reads=[pb[b], yaccB[hp][blk]], writes=[yaccB[hp][blk]])

                pend = [None]
                for g in range(NF // FG):
                    wg, bwg = ws.get(("cols", "moe_w_gate", (j, ex), g * FG * P, FG * P))
                    wu, bwu = ws.get(("cols", "moe_w_up", (j, ex), g * FG * P, FG * P))
                    wd, bwd = ws.get(("rows", "moe_w_down", (j, ex), g * FG * P, FG))
                    for hp in range(2):
                        actpar[0] ^= 1
                        ka = actpar[0]
                        for f in range(FG):
                            k = flip()
                            bg = ctx.ps_next()
                            for kk in range(KC):
                                ctx.op("pe", lambda e: e.matmul(ps[:, bg, 0:CAP], lhsT=wg[:, kk, f * P:(f + 1) * P], rhs=hg[:, hp, kk, :], start=(kk == 0), stop=(kk == 7)),
                                       reads=[bwg, hgb[hp]], writes=[pb[bg]], inc=(kk == 7))
                            bu = ctx.ps_next()
                            for kk in range(KC):
                                ctx.op("pe", lambda e: e.matmul(ps[:, bu, 0:CAP], lhsT=wu[:, kk, f * P:(f + 1) * P], rhs=hg[:, hp, kk, :], start=(kk == 0), stop=(kk == 7)),
                                       reads=[bwu, hgb[hp]], writes=[pb[bu]], inc=(kk == 7))
                            ctx.op("act", lambda e: e.activation(out=sg[:, k, :], in_=ps[:, bg, 0:CAP], func=AF.Silu), reads=[pb[bg]], writes=[sgb[k]])
                            ctx.op("dve", lambda e: e.tensor_tensor(out=actb[:, ka, f, :], in0=ps[:, bu, 0:CAP], in1=sg[:, k, :], op=ALU.mult),
                                   reads=[pb[bu], sgb[k]], writes=[actB[ka]])
                        if pend[0] is not None:
                            emit_down(*pend[0])
                        pend[0] = (g, hp, ka, wd, bwd)
                rows_build(ex, 0)
                emit_down(*pend[0])
                pend[0] = None

            gather(0, 0)
            gather(0, 1)
            for ex in range(NE):
                ffn(ex)
                if ex + 1 < NE:
                    gather(ex + 1, 1)
                scatter(ex, 0)
                rows_build(ex, 1)
                scatter(ex, 1)
                if ex + 1 < NE:
                    gather(ex + 1, 0)
            ctx.barrier()

    def moe(j, l):
        with ExitStack() as ph:
            loc = ffn_locals(ph)
            hf = sb(ph, "hf", [P, KC, P], F32)
            hfb = Buf()
            Gt = sb(ph, "Gt", [P, 2, S], F32)
            Gb = [Buf(), Buf()]
            Dg = sb(ph, "Dg", [P, NB, P], F32)
            Dgb = Buf()
            rt = sb(ph, "rt", [P, 6, NB, NE], F32)
            rs_ = sb(ph, "rs_", [P, 6, NB], F32)
            rb = Buf()
            lgb = ctx.ps_next()
            ctx.reserved.add(lgb)
            rmsnorm("nf", l, router=(j, hf, hfb, lgb))
            L, EQ1, L2, EQ2, GT, TMP = (rt[:, i, :, :] for i in range(6))
            M1, M2, DD, ED, G1, G2 = (rs_[:, i, :] for i in range(6))
            bc = lambda a: a.unsqueeze(2).broadcast_to([P, NB, NE])
            ctx.op("dve", lambda e: e.tensor_copy(out=L, in_=ps[:, lgb, 0:NB * NE].rearrange("p (a b) -> p a b", b=NE)), reads=[pb[lgb]], writes=[rb])
            ctx.reserved.discard(lgb)
            seq = [
                lambda e: e.tensor_reduce(out=M1, in_=L, axis=AX.X, op=ALU.max),
                lambda e: e.tensor_tensor(out=EQ1, in0=L, in1=bc(M1), op=ALU.is_equal),
                lambda e: e.scalar_tensor_tensor(out=L2, in0=EQ1, scalar=-1e30, in1=L, op0=ALU.mult, op1=ALU.add),
                lambda e: e.tensor_reduce(out=M2, in_=L2, axis=AX.X, op=ALU.max),
                lambda e: e.tensor_tensor(out=EQ2, in0=L2, in1=bc(M2), op=ALU.is_equal),
                lambda e: e.tensor_tensor(out=DD, in0=M2, in1=M1, op=ALU.subtract),
            ]
            for fn in seq:
                ctx.op("dve", fn, reads=[rb], writes=[rb])
            ctx.op("act", lambda e: e.activation(out=ED, in_=DD, func=AF.Exp), reads=[rb], writes=[rb])
            seq = [
                lambda e: e.tensor_scalar(out=G2, in0=ED, scalar1=1.0, scalar2=None, op0=ALU.add),
                lambda e: e.reciprocal(out=G1, in_=G2),
                lambda e: e.tensor_tensor(out=G2, in0=ED, in1=G1, op=ALU.mult),
                lambda e: e.tensor_tensor(out=GT, in0=EQ1, in1=bc(G1), op=ALU.mult),
                lambda e: e.tensor_tensor(out=TMP, in0=EQ2, in1=bc(G2), op=ALU.mult),
                lambda e: e.tensor_tensor(out=GT, in0=GT, in1=TMP, op=ALU.add),
            ]
            for fn in seq:
                ctx.op("dve", fn, reads=[rb], writes=[rb])
            for ex in range(NE):
                ke = ex % 2
                ctx.op("dve", lambda e: e.tensor_tensor(out=Dg[:, :, :], in0=cf[:, 0:1, :].broadcast_to([P, NB, P]),
                                                        in1=rt[:, 4, :, ex:ex + 1].broadcast_to([P, NB, P]), op=ALU.mult),
                       reads=[rb, cB], writes=[Dgb])
                for tt in range(NT):
                    b = ctx.ps_next()
                    ctx.op("pe", lambda e: e.matmul(ps[:, b, :], lhsT=cf[:, 1, :], rhs=Dg[:, tt * 4:(tt + 1) * 4, :].rearrange("p a b -> p (a b)"), start=True, stop=True),
                           reads=[Dgb, cB], writes=[pb[b]])
                    ctx.op("act", lambda e: e.copy(out=Gt[:, ke, tts(tt)], in_=ps[:, b, :]), reads=[pb[b]], writes=[Gb[ke]])
                swiglu(("moe_w_gate", "moe_w_up", "moe_w_down"), (j, ex), loc, G=Gt[:, ke, :], Gb=Gb[ke])
            ctx.barrier()

    try:
        ckpt(1)
        for l in layers:
            j = l // 2
            only = getattr(cfg, "only", None)
            if l % 2 == 0:
                if only in (None, "mixer"):
                    even_mixer(j, l)
                if only in (None, "ffn"):
                    dense_ffn(j, l)
            else:
                if only in (None, "mixer"):
                    odd_mixer(j, l)
                if only in (None, "ffn"):
                    (moe_sparse if cfg.sparse else moe)(j, l)
    except StopEmit:
        pass
    if ctx.skip:
        ctx.skip = False
        ctx.barrier()

    with ExitStack() as ph:
        xo = [sb(ph, "xo%d" % i, [P, D], F32) for i in range(2)]
        xob = [Buf(), Buf()]
        osem = [ctx.new_sem(("xo", i)) for i in range(2)]
        for i in range(NB):
            k = i % 2
            for half in range(2):
                b = ctx.ps_next()
                for c4 in range(4):
                    c = half * 4 + c4
                    ctx.op("pe", lambda e: e.transpose(out=ps[:, b, c4 * P:(c4 + 1) * P], in_=xT[:, c, i * P:(i + 1) * P], identity=cf[:, 0, :]),
                           reads=[xb[i // 4], cB], writes=[pb[b]], inc=(c4 == 3))
                if half == 0:
                    ctx.op("act", lambda e: e.copy(out=xo[k][:, 0:512], in_=ps[:, b, :]), reads=[pb[b]], writes=[xob[k]])
                else:
                    ctx.op("dve", lambda e: e.tensor_copy(out=xo[k][:, 512:1024], in_=ps[:, b, :]), reads=[pb[b]], writes=[xob[k]])
            ctx.dma("sp", out=out_d[i * P:(i + 1) * P, :], in_=xo[k][:, :], sem_key=osem[k], reads=[xob[k]])
        if not ctx.dry:
            for kk in osem:
                nc.sync.wait_ge(ctx.sem[kk], ctx.cnt[kk])
    es.close()
    return ws.rec


def build(cfg, layers):
    nc0 = bass.Bass("TRN2", target_bir_lowering=False)
    specs = emit(nc0, Ctx(nc0, True), cfg, layers, None)
    nc = bass.Bass("TRN2", target_bir_lowering=False)
    ctx = Ctx(nc, False)
    emit(nc, ctx, cfg, layers, specs)
    return nc, ctx


def kernel(**inputs):
    cfg = Cfg()
    x = np.asarray(inputs["x"], np.float32)
    sh = prep_shared(cfg, inputs)
    nc, _ = build(cfg, list(range(cfg.depth)))
    in_maps = []
    for b in range(8):
        m = dict(sh)
        m["x"] = np.ascontiguousarray(x[b])
        in_maps.append(m)
    res = run_bass_kernel_spmd(nc, in_maps, core_ids=list(range(8)))
    return np.stack([np.asarray(r["out"], np.float32) for r in res.results], axis=0)
```
